# Optimizing a Trainium2 kernel written in Bass

```python
import math
import jax, jax.numpy as jnp
from jax import lax
import numpy as np

D_MODEL = 1024
BATCH = 8
SEQ = 4096
DEPTH = 2

NORM_EPS = 1e-6
MLA_HEADS = 8
QK_NOPE_DIM = 64
QK_ROPE_DIM = 32
QK_HEAD_DIM = QK_NOPE_DIM + QK_ROPE_DIM
V_HEAD_DIM = 64
Q_LORA_RANK = 256
KV_LORA_RANK = 128
ROPE_THETA = 10000.0
Q_BLOCK = 128
S5_WIDTH = D_MODEL // 2
S5_GROUP = 16
S5_GROUPS = S5_WIDTH // S5_GROUP
S5_STATE = 64
S5_DT_MIN = 1e-3
S5_DT_MAX = 1e-1
POOL_WINDOWS = (2, 4, 8, 16)
POOL_WIDTH = D_MODEL // 2
POOL_GROUP = POOL_WIDTH // len(POOL_WINDOWS)
SGU_WIDTH = D_MODEL // 2
SGU_HEADS = 4
SGU_HEAD_DIM = SGU_WIDTH // SGU_HEADS
SGU_CHUNK = 128
N_EXPERTS = 32
TOP_K = 4
EXPERT_FF = D_MODEL
SWIGLU_ALPHA = 1.702
SWIGLU_LIMIT = 7.0
MOE_BLOCK = 128
EVEN_IN = Q_LORA_RANK + KV_LORA_RANK + QK_ROPE_DIM + S5_WIDTH
EVEN_MIX = MLA_HEADS * V_HEAD_DIM + S5_WIDTH
ODD_IN = POOL_WIDTH + 2 * SGU_WIDTH
ODD_MIX = POOL_WIDTH + SGU_WIDTH

kernel_name = "hybrid_mla_s5_pool_sgu_moe_adaln"


def rmsnorm(x, g):
    xf = x.astype(jnp.float32)
    y = xf * lax.rsqrt(jnp.mean(xf * xf, axis=-1, keepdims=True) + NORM_EPS)
    return (y * g.astype(jnp.float32)).astype(x.dtype)


def rope_tables(positions):
    half = QK_ROPE_DIM // 2
    inv_freq = 1.0 / (ROPE_THETA ** (jnp.arange(half, dtype=jnp.float32) / half))
    ang = positions.astype(jnp.float32)[..., None] * inv_freq
    return jnp.cos(ang), jnp.sin(ang)


def apply_rope(x, cos, sin):
    x1, x2 = jnp.split(x, 2, axis=-1)
    cs = cos[:, :, None, :]
    sn = sin[:, :, None, :]
    return jnp.concatenate([x1 * cs - x2 * sn, x1 * sn + x2 * cs], axis=-1).astype(x.dtype)


def causal_attention(q, k, v):
    B, S, H, Dq = q.shape
    nb = S // Q_BLOCK
    scale = Dq ** -0.5
    qb = q.reshape(B, nb, Q_BLOCK, H, Dq).swapaxes(0, 1)
    kpos = jnp.arange(S)

    def one_block(args):
        q_blk, i = args
        s = jnp.einsum('bqhd,bkhd->bhqk', q_blk, k, preferred_element_type=jnp.float32) * scale
        qpos = i * Q_BLOCK + jnp.arange(Q_BLOCK)
        s = jnp.where(kpos[None, :] <= qpos[:, None], s, -jnp.inf)
        p = jax.nn.softmax(s, axis=-1)
        return jnp.einsum('bhqk,bkhd->bqhd', p.astype(v.dtype), v)

    out = lax.map(one_block, (qb, jnp.arange(nb)))
    return out.swapaxes(0, 1).reshape(B, S, H, v.shape[-1])


def mla_mixer(q_c, kv_c, k_pe, cos, sin, q_norm_g, w_uq, kv_norm_g, w_ukv, q_head_g, k_head_g):
    B, S, _ = q_c.shape
    q = jnp.einsum('bsr,rhd->bshd', rmsnorm(q_c, q_norm_g), w_uq)
    kv = jnp.einsum('bsr,rhd->bshd', rmsnorm(kv_c, kv_norm_g), w_ukv)
    k_nope, v = kv[..., :QK_NOPE_DIM], kv[..., QK_NOPE_DIM:]
    k_rope = jnp.broadcast_to(k_pe[:, :, None, :], (B, S, MLA_HEADS, QK_ROPE_DIM))
    q = rmsnorm(q, q_head_g)
    k = rmsnorm(jnp.concatenate([k_nope, k_rope], axis=-1), k_head_g)
    q = jnp.concatenate([q[..., :QK_NOPE_DIM], apply_rope(q[..., QK_NOPE_DIM:], cos, sin)], axis=-1)
    k = jnp.concatenate([k[..., :QK_NOPE_DIM], apply_rope(k[..., QK_NOPE_DIM:], cos, sin)], axis=-1)
    return causal_attention(q, k, v).reshape(B, S, MLA_HEADS * V_HEAD_DIM)


def _complex_affine_combine(e1, e2):
    a1r, a1i, b1r, b1i = e1
    a2r, a2i, b2r, b2i = e2
    return (a1r * a2r - a1i * a2i,
            a1r * a2i + a1i * a2r,
            a2r * b1r - a2i * b1i + b2r,
            a2r * b1i + a2i * b1r + b2i)


def s5_mixer(u, a_re, a_im, log_dt, b_re, b_im, c_re, c_im, d_skip, glu_w, glu_b):
    B, S, W = u.shape
    f32 = jnp.float32
    uf = u.astype(f32)
    ug = uf.reshape(B, S, S5_GROUPS, S5_GROUP)
    dt = jnp.exp(log_dt.astype(f32))[:, None]
    lam_re = jnp.minimum(a_re.astype(f32), -1e-4)
    lam_im = a_im.astype(f32)
    mag = jnp.exp(lam_re * dt)
    ab_re = mag * jnp.cos(lam_im * dt)
    ab_im = mag * jnp.sin(lam_im * dt)
    den = lam_re * lam_re + lam_im * lam_im
    num_re = ab_re - 1.0
    f_re = (num_re * lam_re + ab_im * lam_im) / den
    f_im = (ab_im * lam_re - num_re * lam_im) / den
    br = b_re.astype(f32)
    bi = b_im.astype(f32)
    bb_re = f_re[..., None] * br - f_im[..., None] * bi
    bb_im = f_re[..., None] * bi + f_im[..., None] * br
    drive_re = jnp.einsum('bsgi,gpi->bsgp', ug, bb_re)
    drive_im = jnp.einsum('bsgi,gpi->bsgp', ug, bb_im)
    a_seq_re = jnp.broadcast_to(ab_re, (S,) + ab_re.shape)
    a_seq_im = jnp.broadcast_to(ab_im, (S,) + ab_im.shape)

    def scan_one(dr, di):
        _, _, xr, xi = lax.associative_scan(_complex_affine_combine, (a_seq_re, a_seq_im, dr, di), axis=0)
        return xr, xi

    st_re, st_im = jax.vmap(scan_one)(drive_re, drive_im)
    y = (jnp.einsum('gip,bsgp->bsgi', c_re.astype(f32), st_re)
         - jnp.einsum('gip,bsgp->bsgi', c_im.astype(f32), st_im))
    y = y.reshape(B, S, W) + d_skip.astype(f32) * uf
    g = jax.nn.gelu(y)
    out = g * jax.nn.sigmoid(g @ glu_w.astype(f32) + glu_b.astype(f32))
    return out.astype(u.dtype)


def pool_mixer(u, w_pool, pool_scale):
    B, S, _ = u.shape
    uf = u.astype(jnp.float32).reshape(B, S, len(POOL_WINDOWS), POOL_GROUP)
    csum = jnp.cumsum(uf, axis=1)
    t = jnp.arange(S)
    outs = []
    for gi, w in enumerate(POOL_WINDOWS):
        cg = csum[:, :, gi]
        lag = jnp.pad(cg, ((0, 0), (w, 0), (0, 0)))[:, :S]
        cnt = jnp.minimum(t + 1, w).astype(jnp.float32)[None, :, None]
        outs.append((cg - lag) / cnt - uf[:, :, gi])
    pooled = jnp.stack(outs, axis=2)
    mixed = jnp.einsum('bsgi,gio->bsgo', pooled, w_pool.astype(jnp.float32)).reshape(B, S, POOL_WIDTH)
    return (mixed * pool_scale.astype(jnp.float32)).astype(u.dtype)


def sgu_mixer(zu, zv, v_norm_g, w_sp, b_sp):
    B, S, _ = zu.shape
    nc = S // SGU_CHUNK
    u = jax.nn.gelu(zu)
    v = rmsnorm(jax.nn.gelu(zv), v_norm_g)
    v = v.reshape(B, nc, SGU_CHUNK, SGU_HEADS, SGU_HEAD_DIM)
    w_causal = jnp.tril(w_sp)
    mixed = jnp.einsum('hts,bnshd->bnthd', w_causal, v) + b_sp.T[None, None, :, :, None]
    return (u.reshape(B, nc, SGU_CHUNK, SGU_HEADS, SGU_HEAD_DIM) * mixed).reshape(B, S, SGU_WIDTH)


def clamped_swiglu(z):
    gate, lin = jnp.split(z, 2, axis=-1)
    gate = jnp.minimum(gate, SWIGLU_LIMIT)
    lin = jnp.clip(lin, -SWIGLU_LIMIT, SWIGLU_LIMIT)
    return gate * jax.nn.sigmoid(SWIGLU_ALPHA * gate) * (lin + 1.0)


def moe_ffn(h, router_w, router_b, w_gu, b_gu, w_dn, b_dn):
    B, S, Dm = h.shape
    x = h.reshape(-1, Dm)
    T = x.shape[0]
    logits = (x @ router_w + router_b).astype(jnp.float32)
    top_logit, top_e = lax.top_k(logits, TOP_K)
    gate = jax.nn.softmax(top_logit, axis=-1)
    flat_e = top_e.reshape(-1)
    flat_tok = jnp.repeat(jnp.arange(T, dtype=jnp.int32), TOP_K)
    flat_gate = gate.reshape(-1)
    order = jnp.argsort(flat_e)
    se, st, sg = flat_e[order], flat_tok[order], flat_gate[order]
    counts = jnp.bincount(flat_e, length=N_EXPERTS)
    padded = (counts + MOE_BLOCK - 1) // MOE_BLOCK * MOE_BLOCK
    pad_end = jnp.cumsum(padded)
    pad_start = pad_end - padded
    start = jnp.cumsum(counts) - counts
    dest = pad_start[se] + (jnp.arange(T * TOP_K) - start[se])
    n_blocks = -(-(T * TOP_K + N_EXPERTS * (MOE_BLOCK - 1)) // MOE_BLOCK)
    n_rows = n_blocks * MOE_BLOCK
    row_tok = jnp.zeros((n_rows,), jnp.int32).at[dest].set(st)
    row_gate = jnp.zeros((n_rows,), jnp.float32).at[dest].set(sg)
    block_e = jnp.minimum(jnp.searchsorted(pad_end, jnp.arange(n_blocks) * MOE_BLOCK, side='right'), N_EXPERTS - 1)
    xb = x[row_tok].reshape(n_blocks, MOE_BLOCK, Dm)

    def expert_block(args):
        xblk, e = args
        z = xblk @ w_gu[e] + b_gu[e]
        return clamped_swiglu(z) @ w_dn[e] + b_dn[e]

    yb = lax.map(expert_block, (xb, block_e)).reshape(n_rows, Dm)
    out = jnp.zeros((T, Dm), jnp.float32).at[row_tok].add(yb.astype(jnp.float32) * row_gate[:, None])
    return out.astype(h.dtype).reshape(B, S, Dm)


def setup_inputs(seed: int = 0) -> dict:
    key = jax.random.key(seed)
    ks = iter(jax.random.split(key, 64))
    f32 = jnp.float32
    D = D_MODEL
    ne = (DEPTH + 1) // 2
    no = DEPTH // 2

    def nrm(shape, s):
        return jax.random.normal(next(ks), shape, f32) * s

    def gain(shape):
        return 1.0 + nrm(shape, 0.1)

    x = nrm((BATCH, SEQ, D), 1.0)
    c = nrm((BATCH, D), 1.0)
    positions = (jax.random.randint(next(ks), (BATCH, 1), 0, 1024, jnp.int32)
                 + jnp.arange(SEQ, dtype=jnp.int32)[None, :])
    return {
        "x": x, "c": c, "positions": positions,
        "ada_w": nrm((DEPTH, D, 6 * D), 0.5 * D ** -0.5),
        "ada_b": nrm((DEPTH, 6 * D), 0.02),
        "norm_mix_g": gain((DEPTH, D)),
        "norm_ffn_g": gain((DEPTH, D)),
        "router_w": nrm((DEPTH, D, N_EXPERTS), D ** -0.5),
        "router_b": nrm((DEPTH, N_EXPERTS), 0.01),
        "moe_w_gu": nrm((DEPTH, N_EXPERTS, D, 2 * EXPERT_FF), D ** -0.5),
        "moe_b_gu": nrm((DEPTH, N_EXPERTS, 2 * EXPERT_FF), 0.01),
        "moe_w_dn": nrm((DEPTH, N_EXPERTS, EXPERT_FF, D), EXPERT_FF ** -0.5),
        "moe_b_dn": nrm((DEPTH, N_EXPERTS, D), 0.01),
        "even_w_in": nrm((ne, D, EVEN_IN), D ** -0.5),
        "mla_q_norm_g": gain((ne, Q_LORA_RANK)),
        "mla_w_uq": nrm((ne, Q_LORA_RANK, MLA_HEADS, QK_HEAD_DIM), Q_LORA_RANK ** -0.5),
        "mla_kv_norm_g": gain((ne, KV_LORA_RANK)),
        "mla_w_ukv": nrm((ne, KV_LORA_RANK, MLA_HEADS, QK_NOPE_DIM + V_HEAD_DIM), KV_LORA_RANK ** -0.5),
        "mla_q_head_g": gain((ne, QK_HEAD_DIM)),
        "mla_k_head_g": gain((ne, QK_HEAD_DIM)),
        "s5_a_re": -0.5 * (1.0 + nrm((ne, S5_GROUPS, S5_STATE), 0.01)),
        "s5_a_im": math.pi * jnp.arange(S5_STATE, dtype=f32) + nrm((ne, S5_GROUPS, S5_STATE), 0.01),
        "s5_log_dt": jax.random.uniform(next(ks), (ne, S5_GROUPS), f32, math.log(S5_DT_MIN), math.log(S5_DT_MAX)),
        "s5_b_re": nrm((ne, S5_GROUPS, S5_STATE, S5_GROUP), (2 * S5_GROUP) ** -0.5),
        "s5_b_im": nrm((ne, S5_GROUPS, S5_STATE, S5_GROUP), (2 * S5_GROUP) ** -0.5),
        "s5_c_re": nrm((ne, S5_GROUPS, S5_GROUP, S5_STATE), S5_STATE ** -0.5),
        "s5_c_im": nrm((ne, S5_GROUPS, S5_GROUP, S5_STATE), S5_STATE ** -0.5),
        "s5_d": nrm((ne, S5_WIDTH), 1.0),
        "s5_glu_w": nrm((ne, S5_WIDTH, S5_WIDTH), S5_WIDTH ** -0.5),
        "s5_glu_b": nrm((ne, S5_WIDTH), 0.02),
        "even_w_out": nrm((ne, EVEN_MIX, D), EVEN_MIX ** -0.5),
        "odd_w_in": nrm((no, D, ODD_IN), D ** -0.5),
        "pool_w": nrm((no, len(POOL_WINDOWS), POOL_GROUP, POOL_GROUP), POOL_GROUP ** -0.5),
        "pool_scale": gain((no, POOL_WIDTH)),
        "sgu_norm_g": gain((no, SGU_WIDTH)),
        "sgu_w": nrm((no, SGU_HEADS, SGU_CHUNK, SGU_CHUNK), SGU_CHUNK ** -0.5),
        "sgu_b": gain((no, SGU_HEADS, SGU_CHUNK)),
        "odd_w_out": nrm((no, ODD_MIX, D), ODD_MIX ** -0.5),
    }


def reference(x, c, positions, ada_w, ada_b, norm_mix_g, norm_ffn_g, router_w, router_b,
              moe_w_gu, moe_b_gu, moe_w_dn, moe_b_dn,
              even_w_in, mla_q_norm_g, mla_w_uq, mla_kv_norm_g, mla_w_ukv, mla_q_head_g, mla_k_head_g,
              s5_a_re, s5_a_im, s5_log_dt, s5_b_re, s5_b_im, s5_c_re, s5_c_im, s5_d, s5_glu_w, s5_glu_b,
              even_w_out,
              odd_w_in, pool_w, pool_scale, sgu_norm_g, sgu_w, sgu_b, odd_w_out):
    cos, sin = rope_tables(positions)
    c_act = jax.nn.silu(c)
    split_even = [Q_LORA_RANK, Q_LORA_RANK + KV_LORA_RANK, Q_LORA_RANK + KV_LORA_RANK + QK_ROPE_DIM]
    split_odd = [POOL_WIDTH, POOL_WIDTH + SGU_WIDTH]
    for layer in range(DEPTH):
        mod = (c_act @ ada_w[layer] + ada_b[layer])[:, None, :]
        sh_m, sc_m, g_m, sh_f, sc_f, g_f = jnp.split(mod, 6, axis=-1)
        h = rmsnorm(x, norm_mix_g[layer]) * (1.0 + sc_m) + sh_m
        i = layer // 2
        if layer % 2 == 0:
            z = h @ even_w_in[i]
            q_c, kv_c, k_pe, u_s5 = jnp.split(z, split_even, axis=-1)
            attn = mla_mixer(q_c, kv_c, k_pe, cos, sin, mla_q_norm_g[i], mla_w_uq[i],
                             mla_kv_norm_g[i], mla_w_ukv[i], mla_q_head_g[i], mla_k_head_g[i])
            ssm = s5_mixer(u_s5, s5_a_re[i], s5_a_im[i], s5_log_dt[i], s5_b_re[i], s5_b_im[i],
                           s5_c_re[i], s5_c_im[i], s5_d[i], s5_glu_w[i], s5_glu_b[i])
            mix = jnp.concatenate([attn, ssm.astype(attn.dtype)], axis=-1) @ even_w_out[i]
        else:
            z = h @ odd_w_in[i]
            u_pool, zu, zv = jnp.split(z, split_odd, axis=-1)
            pooled = pool_mixer(u_pool, pool_w[i], pool_scale[i])
            gated = sgu_mixer(zu, zv, sgu_norm_g[i], sgu_w[i], sgu_b[i])
            mix = jnp.concatenate([pooled, gated], axis=-1) @ odd_w_out[i]
        x = x + g_m * mix
        h = rmsnorm(x, norm_ffn_g[layer]) * (1.0 + sc_f) + sh_f
        x = x + g_f * moe_ffn(h, router_w[layer], router_b[layer], moe_w_gu[layer], moe_b_gu[layer],
                              moe_w_dn[layer], moe_b_dn[layer])
    return x
```

```python
import numpy as np
from contextlib import ExitStack
import concourse.bass as bass
import concourse.mybir as mybir
from concourse.bass_utils import run_bass_kernel_spmd

F32 = mybir.dt.float32
F32R = mybir.dt.float32r
I32 = mybir.dt.int32
BF16 = mybir.dt.bfloat16
U32 = mybir.dt.uint32
AF = mybir.ActivationFunctionType
ALU = mybir.AluOpType

D = 1024
EPS = 1e-6
NE = 32
TWO_PI = 2.0 * np.pi


class View:
    def __init__(self, buf, ap):
        self.buf = buf
        self.ap = ap

    def bc(self, dt):
        return View(self.buf, self.ap.bitcast(dt))

    def re(self, s, **kw):
        return View(self.buf, self.ap.rearrange(s, **kw))

    def __getitem__(self, k):
        return View(self.buf, self.ap[k])


class Buf:
    def __init__(self, t, name, dram=False):
        self.t = t
        self.name = name
        self.dram = dram
        self.lw = {}
        self.rd = {}
        self.dsem = None

    def __getitem__(self, k):
        return View(self, self.t[k])


class Ctx:
    def __init__(self, nc):
        self.nc = nc
        self.engs = {}
        for name in ["tensor", "vector", "scalar", "gpsimd", "sync"]:
            self.engs[name] = dict(h=getattr(nc, name), sem=nc.alloc_semaphore("s_" + name), cnt=0,
                                   waited={}, name=name)
        self.dpool = []
        self.dall = []
        self.nd = 0
        self.phase_bufs = []
        self.nops = 0

    def sb(self, es, name, shape, dt=F32):
        self.nsb = getattr(self, "nsb", 0) + 1
        name = "%s_%d" % (name, self.nsb)
        t = es.enter_context(self.nc.sbuf_tensor(name, list(shape), dt))
        b = Buf(t, name)
        self.phase_bufs.append(b)
        return b

    def ps(self, es, name, shape, dt=F32):
        t = es.enter_context(self.nc.psum_tensor(name, list(shape), dt))
        b = Buf(t, name)
        return b

    def dram(self, name, shape, dt=F32, kind="Internal"):
        t = self.nc.dram_tensor(name, list(shape), dt, kind=kind)
        return Buf(t.ap(), name, dram=True)

    def _dsem(self, b):
        if b.dsem is None:
            if self.dpool:
                b.dsem = self.dpool.pop()
            else:
                self.nd += 1
                d = dict(sem=self.nc.alloc_semaphore("d%d" % self.nd), cnt=0, key="d%d" % self.nd)
                self.dall.append(d)
                b.dsem = d
        return b.dsem

    def _wait(self, E, deps):
        for key, (sem, val) in deps.items():
            if key == "tensor" and E["name"] == "tensor":
                continue
            if E["waited"].get(key, 0) < val:
                E["h"].wait_ge(sem, val)
                E["waited"][key] = val

    @staticmethod
    def _merge(d, key, sem, val):
        if key not in d or d[key][1] < val:
            d[key] = (sem, val)

    def _deps(self, reads, writes):
        deps = {}
        for b in reads:
            for k, (s, v) in b.lw.items():
                self._merge(deps, k, s, v)
        for b in writes:
            for k, (s, v) in b.lw.items():
                self._merge(deps, k, s, v)
            for k, (s, v) in b.rd.items():
                self._merge(deps, k, s, v)
        return deps

    def _record(self, reads, writes, key, sem, val):
        for b in reads:
            self._merge(b.rd, key, sem, val)
        for b in writes:
            if b.dram:
                self._merge(b.lw, key, sem, val)
            else:
                b.lw = {key: (sem, val)}
                b.rd = {}

    def op(self, eng, fn, reads=(), writes=()):
        E = self.engs[eng]
        self._wait(E, self._deps(reads, writes))
        inst = fn(E["h"])
        E["cnt"] += 1
        inst.then_inc(E["sem"], 1)
        self._record(reads, writes, eng, E["sem"], E["cnt"])
        self.nops += 1
        return inst

    def dma(self, out, in_, q="sync", **kw):
        E = self.engs[q]
        self._wait(E, self._deps([in_.buf], [out.buf]))
        sb = in_.buf if out.buf.dram else out.buf
        d = self._dsem(sb)
        inst = E["h"].dma_start(out=out.ap, in_=in_.ap, **kw)
        d["cnt"] += 16
        inst.then_inc(d["sem"], 16)
        self._record([in_.buf], [out.buf], d["key"], d["sem"], d["cnt"])
        self.nops += 1
        return inst

    def gather(self, out, src, offs):
        E = self.engs["gpsimd"]
        self._wait(E, self._deps([src.buf, offs.buf], [out.buf]))
        d = self._dsem(out.buf)
        inst = E["h"].indirect_dma_start(out=out.ap, out_offset=None, in_=src.ap,
                                         in_offset=bass.IndirectOffsetOnAxis(ap=offs.ap, axis=0))
        d["cnt"] += 16
        inst.then_inc(d["sem"], 16)
        self._record([src.buf, offs.buf], [out.buf], d["key"], d["sem"], d["cnt"])
        self.nops += 1
        return inst

    def scatter(self, dst, src, offs):
        E = self.engs["gpsimd"]
        self._wait(E, self._deps([src.buf, offs.buf], [dst.buf]))
        d = self._dsem(src.buf)
        inst = E["h"].indirect_dma_start(out=dst.ap, out_offset=bass.IndirectOffsetOnAxis(ap=offs.ap, axis=0),
                                         in_=src.ap, in_offset=None)
        d["cnt"] += 16
        inst.then_inc(d["sem"], 16)
        self._record([src.buf, offs.buf], [dst.buf], d["key"], d["sem"], d["cnt"])
        self.nops += 1
        return inst

    def barrier(self):
        deps = {}
        for n, X in self.engs.items():
            if X["cnt"] > 0:
                deps[n] = (X["sem"], X["cnt"])
        for d in self.dall:
            if d["cnt"] > 0:
                deps[d["key"]] = (d["sem"], d["cnt"])
        for n, E in self.engs.items():
            for key, (sem, val) in deps.items():
                if E["waited"].get(key, 0) < val:
                    E["h"].wait_ge(sem, val)
                    E["waited"][key] = val
        for b in self.phase_bufs:
            if b.dsem is not None:
                self.dpool.append(b.dsem)
                b.dsem = None
        self.phase_bufs = []

    @staticmethod
    def _rb(*xs):
        return [x.buf for x in xs if isinstance(x, View)]

    @staticmethod
    def _a(x):
        return x.ap if isinstance(x, View) else x

    def mm(self, out, lhsT, rhs, start=True, stop=True):
        return self.op("tensor", lambda e: e.matmul(out.ap, lhsT.ap, rhs.ap, start=start, stop=stop),
                       [lhsT.buf, rhs.buf], [out.buf])

    def transpose(self, out, in_, ident):
        return self.op("tensor", lambda e: e.transpose(out.ap, in_.ap, ident.ap), [in_.buf, ident.buf], [out.buf])

    def act(self, out, in_, func, bias=None, scale=1.0, eng="scalar"):
        kw = {}
        if bias is not None:
            kw["bias"] = self._a(bias)
        kw["scale"] = self._a(scale)
        return self.op("scalar", lambda e: e.activation(out.ap, in_.ap, func, **kw),
                       self._rb(in_, bias, scale), [out.buf])

    def ts(self, out, in0, s1, s2=None, op0=ALU.mult, op1=None, eng="vector"):
        if op1 is None:
            f = lambda e: e.tensor_scalar(out.ap, in0.ap, self._a(s1), None, op0=op0)
        else:
            f = lambda e: e.tensor_scalar(out.ap, in0.ap, self._a(s1), self._a(s2), op0=op0, op1=op1)
        return self.op(eng, f, self._rb(in0, s1, s2), [out.buf])

    def tt(self, out, a, b, op, eng="vector"):
        return self.op(eng, lambda e: e.tensor_tensor(out.ap, a.ap, b.ap, op=op), [a.buf, b.buf], [out.buf])

    def stt(self, out, in0, scalar, in1, op0, op1):
        return self.op("vector", lambda e: e.scalar_tensor_tensor(out.ap, in0.ap, self._a(scalar), in1.ap,
                                                                  op0=op0, op1=op1),
                       self._rb(in0, scalar, in1), [out.buf])

    def copy(self, out, in_, eng="vector"):
        if eng == "scalar":
            return self.op("scalar", lambda e: e.activation(out.ap, in_.ap, AF.Identity), [in_.buf], [out.buf])
        return self.op(eng, lambda e: e.tensor_copy(out.ap, in_.ap), [in_.buf], [out.buf])

    def memset(self, out, val, eng="vector"):
        return self.op(eng, lambda e: e.memset(out.ap, val), [], [out.buf])

    def memset_r(self, out, val):
        self.memset(out.bc(F32), val)
        return self.copy(out, out.bc(F32))

    def recip(self, out, in_):
        return self.op("vector", lambda e: e.reciprocal(out.ap, in_.ap), [in_.buf], [out.buf])

    def scan(self, out, d0, d1, init=0.0):
        return self.op("vector", lambda e: e.tensor_tensor_scan(out.ap, d0.ap, d1.ap, self._a(init),
                                                                op0=ALU.mult, op1=ALU.add),
                       self._rb(d0, d1, init), [out.buf])

    def max_index(self, out, mx, vals):
        return self.op("vector", lambda e: e.max_index(out.ap, mx.ap, vals.ap), [mx.buf, vals.buf], [out.buf])

    def max8(self, out, in_):
        return self.op("vector", lambda e: e.max(out.ap, in_.ap), [in_.buf], [out.buf])

    def rsum(self, out, in_):
        return self.op("vector", lambda e: e.reduce_sum(out.ap, in_.ap, axis=mybir.AxisListType.X),
                       [in_.buf], [out.buf])


class Ring:
    def __init__(self, bufs):
        self.bufs = bufs
        self.i = 0

    def next(self):
        b = self.bufs[self.i % len(self.bufs)]
        self.i += 1
        return b


def range_reduce_sin(c, es, ang, out_sin, out_cos, shape, tagname):
    kf = c.sb(es, tagname + "_kf", shape)
    ki = c.sb(es, tagname + "_ki", shape, I32)
    r = c.sb(es, tagname + "_r", shape)
    m = c.sb(es, tagname + "_m", shape)
    sl = tuple(slice(None) for _ in shape)
    c.ts(kf[sl], ang, 1.0 / TWO_PI)
    c.copy(ki[sl], kf[sl])
    c.copy(kf[sl], ki[sl])
    c.stt(r[sl], kf[sl], -TWO_PI, ang, ALU.mult, ALU.add)

    def wrap(v):
        c.ts(m[sl], v, np.pi, TWO_PI, ALU.is_gt, ALU.mult)
        c.tt(v, v, m[sl], ALU.subtract)
        c.ts(m[sl], v, -np.pi, TWO_PI, ALU.is_lt, ALU.mult)
        c.tt(v, v, m[sl], ALU.add)

    wrap(r[sl])
    c.act(out_sin, r[sl], AF.Sin)
    c.ts(r[sl], r[sl], np.pi / 2, None, ALU.add)
    wrap(r[sl])
    c.act(out_cos, r[sl], AF.Sin)


def gelu_tanh(c, out, x, t1, t2):
    c.act(t1, x, AF.Square)
    c.ts(t1, t1, 0.044715, 1.0, ALU.mult, ALU.add)
    c.tt(t1, t1, x, ALU.mult)
    c.act(t2, t1, AF.Sigmoid, scale=1.5957691216057308)
    c.tt(out, x, t2, ALU.mult)


def gelu_tanh_n(c, outs, xs, t1s, t2s):
    n = len(outs)
    for i in range(n):
        c.act(t1s[i], xs[i], AF.Square)
    for i in range(n):
        c.ts(t1s[i], t1s[i], 0.044715, 1.0, ALU.mult, ALU.add)
    for i in range(n):
        c.tt(t1s[i], t1s[i], xs[i], ALU.mult)
    for i in range(n):
        c.act(t2s[i], t1s[i], AF.Sigmoid, scale=1.5957691216057308)
    for i in range(n):
        c.tt(outs[i], xs[i], t2s[i], ALU.mult)


def build(S, debug=False, phases=None, odd_src=None, moe_src=None, moe_mode="sparse"):
    NT = S // 512
    allph = ["ada", "e1", "attn", "s5", "e4", "moe0", "odd", "moe1"]
    phases = allph if phases is None else phases
    nc = bass.Bass("TRN2", target_bir_lowering=False)
    c = Ctx(nc)
    skind = "ExternalOutput" if debug else "Internal"

    def din(name, shape, dt=F32):
        return c.dram(name, shape, dt, kind="ExternalInput")

    def dscr(name, shape):
        return c.dram(name, shape, F32, kind=skind)

    xT = din("xT", [D, S])
    cT = din("cT", [128, 8])
    pos = din("pos", [1, S], I32)
    ada_w = din("ada_w", [2, D, 6 * D])
    ada_bT = din("ada_bT", [128, 96])
    nmg = din("nmg", [128, 16])
    nfg = din("nfg", [128, 16])
    router_w = din("router_w", [2, D, NE])
    router_b = din("router_b", [2, NE])
    wgu = din("wgu", [2 * NE * 4 * 128, 4096])
    bgu = din("bgu", [2, 128, NE, 16])
    wdn = din("wdn", [2 * NE * 2 * 128, 4096])
    bdn = din("bdn", [2, NE, D])
    e_win = din("e_win", [D, 928])
    qng = din("qng", [128, 2])
    kvng = din("kvng", [128, 1])
    wuq = din("wuq", [8, 128, 2, 96])
    wuqs = din("wuqs", [8, 128, 2, 96])
    wk = din("wk", [8, 128, 64])
    wv = din("wv", [8, 128, 64])
    qhg = din("qhg", [96, 2])
    khg = din("khg", [96, 2])
    ident_d = din("ident", [128, 128])
    ipe_d = din("ipe", [32, 2, 96])
    ropec = din("ropec", [96, 2])
    sel_d = din("sel", [32, NE, 128])
    sel65_d = din("sel65", [65, 64])
    cmask_d = din("cmask", [128, 4, 512])
    inv16_d = din("inv16", [128, 4, 16])
    s5p_d = din("s5p", [128, 3, 16])
    s5b_d = din("s5b", [128, 2, 16, 16])
    s5c_d = din("s5c", [128, 2, 16, 16])
    s5d_d = din("s5d", [128, 4])
    gluw_d = din("gluw", [512, 512])
    glub_d = din("glub", [128, 4])
    e_wout = din("e_wout", [D, D])
    o_win = din("o_win", [D, 1536])
    poolw_d = din("poolw", [128, 4, 128])
    pools_d = din("pools", [128, 4])
    sgug_d = din("sgug", [128, 512])
    sguw_d = din("sguw", [128, 4, 128])
    sgub_d = din("sgub", [1, 512])
    o_wout = din("o_wout", [D, D])
    outT = c.dram("outT", [D, S], F32, kind="ExternalOutput")
    NSUB = S // 128
    NBLK = (4 * S + NE * 511 + 511) // 512
    NROWS = NBLK * 512
    bgu2 = din("bgu2", [2, NE * 128, 16])
    bdn2 = din("bdn2", [2, NE * 128, 8])
    bdnr_d = din("bdnr", [2 * NE, D])
    ustrict_d = din("ustrict", [128, 128])
    jvec_d = din("jvec", [128, NBLK])
    eiota_d = din("eiota", [128, NE])
    tokid_d = din("tokid", [128, NSUB])
    widx_d = din("widx", [128, 9])
    rowinit_d = din("rowinit", [128, NBLK * 4, 2])
    hrows = dscr("hrows", [S + 512, D])
    rowinfo = dscr("rowinfo", [NROWS, 2])
    yrows = dscr("yrows", [NROWS, D])

    qnT = dscr("qnT", [256, S])
    kvnT = dscr("kvnT", [128, S])
    kpeT = dscr("kpeT", [32, S])
    uT = dscr("uT", [512, S])
    mixT = dscr("mixT", [D, S])
    x1T = dscr("x1T", [D, S])
    x2T = dscr("x2T", [D, S])
    x3T = dscr("x3T", [D, S])
    modD = dscr("modD", [128, 96])

    def fm(dv, i, nch):
        return dv[:, i * 512:(i + 1) * 512].re("(kc p) t -> p kc t", p=128)

    with ExitStack() as es0:
        P = [c.ps(es0, "P%d" % i, [128, 512]) for i in range(8)]
        ident = c.sb(es0, "ident_sb", [128, 128])
        ones_r = c.sb(es0, "ones_r", [128, 128], F32R)
        ones_f = c.sb(es0, "ones_f", [128, 128])
        mod = c.sb(es0, "mod", [128, 96])
        gg = c.sb(es0, "gg", [128, 4, 8])
        c.phase_bufs = []
        c.dma(ident[:], ident_d[:])
        c.memset_r(ones_r[:], 1.0)
        c.memset(ones_f[:], 1.0)

        if "ada" in phases:
            with ExitStack() as es:
                ct = c.sb(es, "ct", [128, 8])
                sig = c.sb(es, "sig", [128, 8])
                cact = c.sb(es, "cact", [128, 8, 2])
                adab = c.sb(es, "adab", [128, 96])
                nm = c.sb(es, "nm", [128, 16])
                nf = c.sb(es, "nf", [128, 16])
                wr = Ring([c.sb(es, "adaw%d" % i, [128, 8, 1024]) for i in range(2)])
                c.dma(ct[:], cT[:])
                c.dma(adab[:], ada_bT[:])
                c.dma(nm[:], nmg[:])
                c.dma(nf[:], nfg[:])
                c.act(sig[:], ct[:], AF.Sigmoid)
                c.tt(cact[:, :, 0], ct[:], sig[:], ALU.mult)
                c.tt(cact[:, :, 1], ct[:], sig[:], ALU.mult)
                for l in range(2):
                    for j in range(6):
                        wt = wr.next()
                        c.dma(wt[:], ada_w[l, :, j * 1024:(j + 1) * 1024].re("(kc p) f -> p kc f", p=128))
                        for fi in range(8):
                            idx = l * 48 + j * 8 + fi
                            for kc in range(8):
                                c.mm(P[0][:, idx * 2:idx * 2 + 2], wt[:, kc, fi * 128:(fi + 1) * 128],
                                     cact[:, kc, :], start=(kc == 0), stop=(kc == 7))
                c.tt(mod[:], P[0][:, 0:192].re("p (a b) -> p a b", b=2)[:, :, 0], adab[:], ALU.add)
                for l in range(2):
                    c.stt(gg[:, l * 2 + 0, :], mod[:, l * 48 + 8:l * 48 + 16], 1.0, nm[:, l * 8:(l + 1) * 8],
                          ALU.add, ALU.mult)
                    c.stt(gg[:, l * 2 + 1, :], mod[:, l * 48 + 32:l * 48 + 40], 1.0, nf[:, l * 8:(l + 1) * 8],
                          ALU.add, ALU.mult)
                if debug:
                    c.dma(modD[:], mod[:])
                c.barrier()

        def mk_norm(es, tag):
            sq = c.sb(es, tag + "_sq", [128, 8, 512], F32R)
            rs = c.sb(es, tag + "_rs", [128, 512])
            tr = Ring([c.sb(es, tag + "_t%d" % i, [128, 512]) for i in range(2)])
            return sq, rs, tr

        def rstd_from(rs, pv, n, npart=128):
            c.ts(rs[0:npart, :], pv, 1.0 / n, EPS, ALU.mult, ALU.add)
            c.act(rs[0:npart, :], rs[0:npart, :], AF.Ln)
            c.act(rs[0:npart, :], rs[0:npart, :], AF.Exp, scale=-0.5)

        def norm_tile(st, xt, hout, ggv, shv, pbank):
            sq, rs, tr = st
            for kc in range(8):
                c.act(sq[:, kc, :], xt[:, kc, :], AF.Square)
            for kc in range(8):
                c.mm(pbank[:, :], ones_r[:, :], sq[:, kc, :], start=(kc == 0), stop=(kc == 7))
            rstd_from(rs, pbank[:, :], float(D))
            for kc in range(8):
                t = tr.next()
                c.stt(t[:, :], xt[:, kc, :], ggv[:, kc:kc + 1], rs[:, :], ALU.mult, ALU.mult)
                c.act(hout[:, kc, :], t[:, :], AF.Identity, bias=shv[:, kc:kc + 1])

        if "e1" in phases:
            with ExitStack() as es:
                win = c.sb(es, "win", [128, 8, 928], F32R)
                c.dma(win[:], e_win[:, :].re("(kc p) f -> p kc f", p=128), q="gpsimd")
                qg = c.sb(es, "qg", [128, 2])
                kg = c.sb(es, "kg", [128, 1])
                c.dma(qg[:], qng[:])
                c.dma(kg[:], kvng[:])
                st = mk_norm(es, "n1")
                xr_ = Ring([c.sb(es, "e1x%d" % i, [128, 8, 512]) for i in range(2)])
                h = c.sb(es, "e1h", [128, 8, 512], F32R)
                sq2 = c.sb(es, "e1sq2", [128, 2, 512], F32R)
                rs2 = c.sb(es, "e1rs2", [128, 512])
                qn_sb = c.sb(es, "e1qn", [128, 2, 512])
                kvn_sb = c.sb(es, "e1kvn", [128, 512])
                kpe_sb = c.sb(es, "e1kpe", [32, 512])
                u_sb = c.sb(es, "e1u", [128, 4, 512])
                for i in range(NT):
                    xt = xr_.next()
                    c.dma(xt[:], fm(xT, i, 8))
                    norm_tile(st, xt, h, gg[:, 0, :], mod[:, 0:8], P[4])
                    for m in range(2):
                        for kc in range(8):
                            c.mm(P[m][:, :], win[:, kc, m * 128:(m + 1) * 128], h[:, kc, :], start=(kc == 0), stop=(kc == 7))
                    for kc in range(8):
                        c.mm(P[2][:, :], win[:, kc, 256:384], h[:, kc, :], start=(kc == 0), stop=(kc == 7))
                    for kc in range(8):
                        c.mm(P[3][:, :], win[:, kc, 384:512], h[:, kc, :], start=(kc == 0), stop=(kc == 7))
                    for m in range(2):
                        c.act(sq2[:, m, :], P[m][:, :], AF.Square)
                    for m in range(2):
                        c.mm(P[5][:, :], ones_r[:, :], sq2[:, m, :], start=(m == 0), stop=(m == 1))
                    rstd_from(rs2, P[5][:, :], 256.0)
                    for m in range(2):
                        c.stt(qn_sb[:, m, :], P[m][:, :], qg[:, m:m + 1], rs2[:, :], ALU.mult, ALU.mult)
                    c.dma(fm(qnT, i, 2), qn_sb[:])
                    c.act(sq2[:, 0, :], P[2][:, :], AF.Square)
                    c.mm(P[5][:, :], ones_r[:, :], sq2[:, 0, :])
                    rstd_from(rs2, P[5][:, :], 128.0)
                    c.stt(kvn_sb[:, :], P[2][:, :], kg[:, 0:1], rs2[:, :], ALU.mult, ALU.mult)
                    c.dma(kvnT[:, i * 512:(i + 1) * 512], kvn_sb[:])
                    c.copy(kpe_sb[:, :], P[3][0:32, :], eng="scalar")
                    c.dma(kpeT[:, i * 512:(i + 1) * 512], kpe_sb[:])
                    for m in range(4):
                        pb = P[6 + (m % 2)]
                        for kc in range(8):
                            c.mm(pb[:, :], win[:, kc, 416 + m * 128:416 + (m + 1) * 128], h[:, kc, :],
                                 start=(kc == 0), stop=(kc == 7))
                        c.copy(u_sb[:, m, :], pb[:, :], eng=("scalar" if m % 2 else "vector"))
                    c.dma(fm(uT, i, 4), u_sb[:])
                c.barrier()

        if "attn" in phases:
            with ExitStack() as es:
                cosT = c.sb(es, "cosT", [96, S])
                sinT = c.sb(es, "sinT", [96, S])
                with ExitStack() as es2:
                    posi = c.sb(es2, "posi", [96, S], I32)
                    ang = c.sb(es2, "ang", [96, S])
                    rc = c.sb(es2, "rc", [96, 2])
                    c.dma(rc[:], ropec[:])
                    c.dma(posi[:], View(pos, pos.t[0:1, :].partition_broadcast(96)))
                    c.copy(ang[:], posi[:])
                    c.ts(ang[:], ang[:], rc[:, 0:1])
                    range_reduce_sin(c, es2, ang[:], sinT[:], cosT[:], [96, S], "rr")
                    c.ts(sinT[:], sinT[:], rc[:, 1:2])
                    c.barrier()
                qn = c.sb(es, "a_qn", [128, 2, S], F32R)
                kvn = c.sb(es, "a_kvn", [128, S], F32R)
                kpe = c.sb(es, "a_kpe", [32, S], F32R)
                for m in range(2):
                    c.dma(qn[:, m, :], qnT[m * 128:(m + 1) * 128, :], q="gpsimd", max_dma_last_dim=4096)
                c.dma(kvn[:], kvnT[:], q="gpsimd", max_dma_last_dim=4096)
                c.dma(kpe[:], kpeT[:], q="gpsimd", max_dma_last_dim=4096)
                ipe = c.sb(es, "a_ipe", [32, 2, 96], F32R)
                c.dma(ipe[:], ipe_d[:], q="gpsimd")
                qh = c.sb(es, "a_qh", [96, 2])
                kh = c.sb(es, "a_kh", [96, 2])
                c.dma(qh[:], qhg[:])
                c.dma(kh[:], khg[:])
                cmask = c.sb(es, "a_cmask", [128, 4, 512])
                c.dma(cmask[:], cmask_d[:])
                sel65 = c.sb(es, "a_sel65", [65, 64])
                c.dma(sel65[:], sel65_d[:])
                wq = c.sb(es, "a_wq", [128, 2, 96], F32R)
                wqs = c.sb(es, "a_wqs", [128, 2, 96], F32R)
                wkt = c.sb(es, "a_wk", [128, 96], F32R)
                wvt = c.sb(es, "a_wv", [128, 64], F32R)
                qTt = c.sb(es, "a_qT", [96, S], F32R)
                kTt = c.sb(es, "a_kT", [96, S], F32R)
                V = c.sb(es, "a_V", [128, S // 128, 65], F32R)
                sq96 = c.sb(es, "a_sq96", [96, 512], F32R)
                rs96 = c.sb(es, "a_rs96", [128, 512])
                sq96b = c.sb(es, "a_sq96b", [96, 512], F32R)
                rs96b = c.sb(es, "a_rs96b", [128, 512])
                t1r = Ring([c.sb(es, "a_t1%d" % i, [96, 512]) for i in range(2)])
                t2r = Ring([c.sb(es, "a_t2%d" % i, [96, 512]) for i in range(2)])
                pTr = Ring([c.sb(es, "a_pT%d" % i, [128, 512], F32R) for i in range(4)])
                osb = c.sb(es, "a_osb", [65, 512])
                rec = c.sb(es, "a_rec", [64, 512])
                att = Ring([c.sb(es, "a_att%d" % i, [64, 512]) for i in range(2)])
                scale = 96.0 ** -0.5
                c.memset_r(wkt[:], 0.0)
                c.memset_r(V[:], 1.0)
                for hh in range(8):
                    c.dma(wq[:], wuq[hh], q="gpsimd")
                    c.dma(wqs[:], wuqs[hh], q="gpsimd")
                    c.dma(wkt[:, 0:64], wk[hh], q="gpsimd")
                    c.dma(wvt[:], wv[hh], q="gpsimd")
                    for i in range(NT):
                        tl = slice(i * 512, (i + 1) * 512)
                        for kc in range(2):
                            c.mm(P[0][0:96, :], wq[:, kc, :], qn[:, kc, tl], start=(kc == 0), stop=(kc == 1))
                        for kc in range(2):
                            c.mm(P[1][0:96, :], wqs[:, kc, :], qn[:, kc, tl], start=(kc == 0), stop=(kc == 1))
                        c.mm(P[2][0:96, :], wkt[:, :], kvn[:, tl], start=True, stop=False)
                        c.mm(P[2][0:96, :], ipe[:, 0, :], kpe[:, tl], start=False, stop=True)
                        c.mm(P[3][0:96, :], ipe[:, 1, :], kpe[:, tl], start=True, stop=True)
                        qk = ((P[0], P[1], qh, qTt, sq96, rs96, P[4]), (P[2], P[3], kh, kTt, sq96b, rs96b, P[5]))
                        for (pa, pb, gv, dst, sq_, rs_, pn) in qk:
                            c.act(sq_[:, :], pa[0:96, :], AF.Square)
                        for (pa, pb, gv, dst, sq_, rs_, pn) in qk:
                            c.mm(pn[0:96, :], ones_r[0:96, 0:96], sq_[:, :])
                        for (pa, pb, gv, dst, sq_, rs_, pn) in qk:
                            c.ts(rs_[0:96, :], pn[0:96, :], 1.0 / 96.0, EPS, ALU.mult, ALU.add)
                        for (pa, pb, gv, dst, sq_, rs_, pn) in qk:
                            c.act(rs_[0:96, :], rs_[0:96, :], AF.Ln)
                        for (pa, pb, gv, dst, sq_, rs_, pn) in qk:
                            c.act(rs_[0:96, :], rs_[0:96, :], AF.Exp, scale=-0.5)
                        tq = []
                        for (pa, pb, gv, dst, sq_, rs_, pn) in qk:
                            t1 = t1r.next()
                            t2 = t2r.next()
                            tq.append((t1, t2))
                            c.stt(t1[:, :], pa[0:96, :], gv[:, 0:1], cosT[:, tl], ALU.mult, ALU.mult)
                            c.stt(t2[:, :], pb[0:96, :], gv[:, 1:2], sinT[:, tl], ALU.mult, ALU.mult)
                        for (pa, pb, gv, dst, sq_, rs_, pn), (t1, t2) in zip(qk, tq):
                            c.tt(t1[:, :], t1[:, :], t2[:, :], ALU.add)
                        for (pa, pb, gv, dst, sq_, rs_, pn), (t1, t2) in zip(qk, tq):
                            c.tt(dst[:, tl], t1[:, :], rs_[0:96, :], ALU.mult)
                    for j in range(S // 128):
                        c.mm(P[5][:, 0:64], kvn[:, j * 128:(j + 1) * 128], wvt[:, :])
                        c.copy(V[:, j, 0:64], P[5][:, 0:64], eng="scalar")
                    units = [(qi, kj) for qi in range(NT) for kj in range(4 * (qi + 1))]
                    sbanks = [P[0], P[1], P[6], P[7]]

                    def issue_s(u):
                        qi_, kj_ = units[u]
                        c.mm(sbanks[u % 4][:, :], kTt[:, kj_ * 128:(kj_ + 1) * 128], qTt[:, qi_ * 512:(qi_ + 1) * 512])

                    for u in range(min(2, len(units))):
                        issue_s(u)
                    for u, (qi, kj) in enumerate(units):
                        if u + 2 < len(units):
                            issue_s(u + 2)
                        ql = slice(qi * 512, (qi + 1) * 512)
                        po = P[4 + (qi % 2)]
                        nk = 4 * (qi + 1)
                        pT = pTr.next()
                        c.act(pT[:, :], sbanks[u % 4][:, :], AF.Exp, scale=scale)
                        if kj >= 4 * qi:
                            c.tt(pT[:, :], pT[:, :].bc(F32), cmask[:, kj - 4 * qi, :], ALU.mult)
                        c.mm(po[0:65, :], V[:, kj, :], pT[:, :], start=(kj == 0), stop=(kj == nk - 1))
                        if kj == nk - 1:
                            c.copy(osb[:, :], po[0:65, :], eng="scalar")
                            c.mm(P[2][0:64, :], sel65[:, :], osb[:, :])
                            c.recip(rec[:, :], P[2][0:64, :])
                            a_ = att.next()
                            c.tt(a_[:, :], osb[0:64, :], rec[:, :], ALU.mult)
                            c.dma(mixT[hh * 64:(hh + 1) * 64, ql], a_[:])
                c.barrier()

        if "s5" in phases:
            with ExitStack() as es:
                sp = c.sb(es, "s_p", [128, 3, 16])
                c.dma(sp[:], s5p_d[:])
                names = ["dt", "lr", "mag", "ang", "sinA", "cosA", "abr", "abi", "nabi", "den", "nr", "fr", "fi",
                         "nfi", "t0", "t1", "pr", "pi", "npi", "t2"]
                v = {n: c.sb(es, "s_" + n, [128, 16]) for n in names}
                A = lambda n: v[n][:, :]
                li = sp[:, 1, :]
                c.act(A("dt"), sp[:, 2, :], AF.Exp)
                c.ts(A("lr"), sp[:, 0, :], -1e-4, None, ALU.min)
                c.tt(A("t0"), A("lr"), A("dt"), ALU.mult)
                c.act(A("mag"), A("t0"), AF.Exp)
                c.tt(A("ang"), li, A("dt"), ALU.mult)
                with ExitStack() as es2:
                    range_reduce_sin(c, es2, A("ang"), A("sinA"), A("cosA"), [128, 16], "srr")
                    c.barrier()
                c.tt(A("abr"), A("mag"), A("cosA"), ALU.mult)
                c.tt(A("abi"), A("mag"), A("sinA"), ALU.mult)
                c.ts(A("nabi"), A("abi"), -1.0)
                c.tt(A("den"), A("lr"), A("lr"), ALU.mult)
                c.tt(A("t0"), li, li, ALU.mult)
                c.tt(A("den"), A("den"), A("t0"), ALU.add)
                c.recip(A("den"), A("den"))
                c.ts(A("nr"), A("abr"), -1.0, None, ALU.add)
                c.tt(A("t0"), A("nr"), A("lr"), ALU.mult)
                c.tt(A("t1"), A("abi"), li, ALU.mult)
                c.tt(A("t0"), A("t0"), A("t1"), ALU.add)
                c.tt(A("fr"), A("t0"), A("den"), ALU.mult)
                c.tt(A("t0"), A("abi"), A("lr"), ALU.mult)
                c.tt(A("t1"), A("nr"), li, ALU.mult)
                c.tt(A("t0"), A("t0"), A("t1"), ALU.subtract)
                c.tt(A("fi"), A("t0"), A("den"), ALU.mult)
                c.ts(A("nfi"), A("fi"), -1.0)
                c.copy(A("pr"), A("cosA"))
                c.ts(A("pi"), A("sinA"), -1.0)
                c.copy(A("npi"), A("sinA"))

                BT = c.sb(es, "s_BT", [128, 16, 2, 128], F32R)
                CP = c.sb(es, "s_CP", [128, 16, 2, 128], F32R)
                Er = c.sb(es, "s_Er", [128, 16, 512])
                Ei = c.sb(es, "s_Ei", [128, 16, 512])
                with ExitStack() as es2:
                    sb_ = c.sb(es2, "s_b", [128, 2, 16, 16])
                    sc_ = c.sb(es2, "s_c", [128, 2, 16, 16])
                    c.dma(sb_[:], s5b_d[:])
                    c.dma(sc_[:], s5c_d[:])
                    bb = c.sb(es2, "s_bb", [128, 2, 16, 16])
                    tmp = c.sb(es2, "s_tmp", [128, 16])
                    BP = c.sb(es2, "s_BP", [128, 16, 2, 128])
                    c.memset(BP[:], 0.0)
                    c.memset_r(CP[:], 0.0)
                    for cc in range(16):
                        c.ts(tmp[:, :], sb_[:, 0, cc, :], v["fr"][:, cc:cc + 1])
                        c.stt(bb[:, 0, cc, :], sb_[:, 1, cc, :], v["nfi"][:, cc:cc + 1], tmp[:, :], ALU.mult, ALU.add)
                        c.ts(tmp[:, :], sb_[:, 1, cc, :], v["fr"][:, cc:cc + 1])
                        c.stt(bb[:, 1, cc, :], sb_[:, 0, cc, :], v["fi"][:, cc:cc + 1], tmp[:, :], ALU.mult, ALU.add)
                        offs = (((2 * cc) % 8) * 16, ((2 * cc + 1) % 8) * 16)
                        for half in range(2):
                            ps_ = slice(half * 64, (half + 1) * 64)
                            o = offs[half]
                            for ri in range(2):
                                c.copy(BP[ps_, cc, ri, o:o + 16], bb[ps_, ri, cc, :])
                            c.copy(CP[ps_, cc, 0, o:o + 16], sc_[ps_, 0, cc, :])
                            c.ts(CP[ps_, cc, 1, o:o + 16], sc_[ps_, 1, cc, :], -1.0)
                        for ri in range(2):
                            pb = P[(cc * 2 + ri) % 4]
                            c.transpose(pb[:, 0:128], BP[:, cc, ri, :], ident[:, :])
                            c.copy(BT[:, cc, ri, :], pb[:, 0:128], eng="scalar")
                    c.barrier()
                c.memset(Er[:, :, 0:1], 1.0)
                c.memset(Ei[:, :, 0:1], 0.0)
                tA = c.sb(es, "s_tA", [128, 256])
                tB = c.sb(es, "s_tB", [128, 256])
                m = 1
                while m < 512:
                    for cc in range(16):
                        c.ts(tA[:, 0:m], Er[:, cc, 0:m], v["pr"][:, cc:cc + 1])
                        c.stt(Er[:, cc, m:2 * m], Ei[:, cc, 0:m], v["npi"][:, cc:cc + 1], tA[:, 0:m], ALU.mult, ALU.add)
                        c.ts(tB[:, 0:m], Er[:, cc, 0:m], v["pi"][:, cc:cc + 1])
                        c.stt(Ei[:, cc, m:2 * m], Ei[:, cc, 0:m], v["pr"][:, cc:cc + 1], tB[:, 0:m], ALU.mult, ALU.add)
                    m *= 2
                    if m < 512:
                        c.tt(A("t0"), A("pr"), A("pr"), ALU.mult)
                        c.tt(A("t1"), A("pi"), A("pi"), ALU.mult)
                        c.tt(A("t2"), A("pr"), A("pi"), ALU.mult)
                        c.tt(A("pr"), A("t0"), A("t1"), ALU.subtract)
                        c.ts(A("pi"), A("t2"), 2.0)
                        c.ts(A("npi"), A("t2"), -2.0)
                dsk = c.sb(es, "s_dsk", [128, 4])
                glub = c.sb(es, "s_glub", [128, 4])
                gluw = c.sb(es, "s_gluw", [128, 4, 512], F32R)
                c.dma(dsk[:], s5d_d[:])
                c.dma(glub[:], glub_d[:])
                c.dma(gluw[:], gluw_d[:, :].re("(kc p) f -> p kc f", p=128), q="gpsimd")
                ones512 = c.sb(es, "s_ones", [128, 512])
                c.memset(ones512[:], 1.0)
                xprev = c.sb(es, "s_xprev", [128, 16, 2])
                utr = Ring([c.sb(es, "s_ut%d" % i, [128, 4, 512], F32R) for i in range(2)])
                tr_ = [Ring([c.sb(es, "s_w%d_%d" % (k, i), [128, 512]) for i in range(2)]) for k in range(7)]
                tr2_ = [Ring([c.sb(es, "s_v%d_%d" % (k, i), [128, 512]) for i in range(2)]) for k in range(2)]
                xrr = Ring([c.sb(es, "s_xr%d" % i, [128, 512], F32R) for i in range(2)])
                xir = Ring([c.sb(es, "s_xi%d" % i, [128, 512], F32R) for i in range(2)])
                cr_ = c.sb(es, "s_cr", [128, 2])
                yy = c.sb(es, "s_yy", [128, 512])
                g1 = c.sb(es, "s_g1", [128, 512])
                g2 = c.sb(es, "s_g2", [128, 512])
                gt = c.sb(es, "s_gt", [128, 4, 512], F32R)
                gf = c.sb(es, "s_gf", [128, 512])
                sg = c.sb(es, "s_sg", [128, 512])
                ot = c.sb(es, "s_ot", [128, 4, 512])
                for n in range(NT):
                    ut = utr.next()
                    c.dma(ut[:], fm(uT, n, 4), q="gpsimd")
                    for cc in range(16):
                        uc = cc // 4
                        pdr, pdi = P[0 + 2 * (cc % 2)], P[1 + 2 * (cc % 2)]
                        c.mm(pdr[:, :], BT[:, cc, 0, :], ut[:, uc, :])
                        c.mm(pdi[:, :], BT[:, cc, 1, :], ut[:, uc, :])
                        a1, a2, b1, b2, rt, sr, si = [r.next() for r in tr_]
                        c.tt(a1[:, :], pdr[:, :], Er[:, cc, :], ALU.mult)
                        c.tt(a2[:, :], pdi[:, :], Ei[:, cc, :], ALU.mult)
                        c.tt(a1[:, :], a1[:, :], a2[:, :], ALU.subtract)
                        c.tt(b1[:, :], pdr[:, :], Ei[:, cc, :], ALU.mult)
                        c.tt(b2[:, :], pdi[:, :], Er[:, cc, :], ALU.mult)
                        c.tt(b1[:, :], b1[:, :], b2[:, :], ALU.add)
                        if n > 0:
                            c.ts(cr_[:, 0:1], xprev[:, cc, 0:1], v["abr"][:, cc:cc + 1])
                            c.stt(cr_[:, 0:1], xprev[:, cc, 1:2], v["nabi"][:, cc:cc + 1], cr_[:, 0:1], ALU.mult, ALU.add)
                            c.ts(cr_[:, 1:2], xprev[:, cc, 0:1], v["abi"][:, cc:cc + 1])
                            c.stt(cr_[:, 1:2], xprev[:, cc, 1:2], v["abr"][:, cc:cc + 1], cr_[:, 1:2], ALU.mult, ALU.add)
                            c.tt(a1[:, 0:1], a1[:, 0:1], cr_[:, 0:1], ALU.add)
                            c.tt(b1[:, 0:1], b1[:, 0:1], cr_[:, 1:2], ALU.add)
                        c.ts(rt[:, :], ones512[:, :], v["mag"][:, cc:cc + 1])
                        c.scan(sr[:, :], rt[:, :], a1[:, :])
                        c.scan(si[:, :], rt[:, :], b1[:, :])
                        xr, xi = xrr.next(), xir.next()
                        a3, b3 = tr2_[0].next(), tr2_[1].next()
                        c.tt(a2[:, :], sr[:, :], Er[:, cc, :], ALU.mult, eng="gpsimd")
                        c.tt(b2[:, :], si[:, :], Ei[:, cc, :], ALU.mult, eng="gpsimd")
                        c.tt(a3[:, :], si[:, :], Er[:, cc, :], ALU.mult, eng="gpsimd")
                        c.tt(b3[:, :], sr[:, :], Ei[:, cc, :], ALU.mult, eng="gpsimd")
                        c.tt(xr[:, :], a2[:, :], b2[:, :], ALU.add)
                        c.tt(xi[:, :], a3[:, :], b3[:, :], ALU.subtract)
                        c.copy(xprev[:, cc, 0:1], xr[:, 511:512].bc(F32))
                        c.copy(xprev[:, cc, 1:2], xi[:, 511:512].bc(F32))
                        py = P[4 + (uc % 2)]
                        c.mm(py[:, :], CP[:, cc, 0, :], xr[:, :], start=(cc % 4 == 0), stop=False)
                        c.mm(py[:, :], CP[:, cc, 1, :], xi[:, :], start=False, stop=(cc % 4 == 3))
                        if cc % 4 == 3:
                            c.stt(yy[:, :], ut[:, uc, :].bc(F32), dsk[:, uc:uc + 1], py[:, :], ALU.mult, ALU.add)
                            gelu_tanh(c, gf[:, :], yy[:, :], g1[:, :], g2[:, :])
                            c.copy(gt[:, uc, :], gf[:, :], eng="scalar")
                    for fo in range(4):
                        pg = P[6 + (fo % 2)]
                        for kc in range(4):
                            c.mm(pg[:, :], gluw[:, kc, fo * 128:(fo + 1) * 128], gt[:, kc, :], start=(kc == 0), stop=(kc == 3))
                        c.act(sg[:, :], pg[:, :], AF.Sigmoid, bias=glub[:, fo:fo + 1])
                        c.tt(ot[:, fo, :], gt[:, fo, :].bc(F32), sg[:, :], ALU.mult)
                    c.dma(mixT[512:1024, n * 512:(n + 1) * 512].re("(kc p) t -> p kc t", p=128), ot[:])
                c.barrier()

        if "e4" in phases:
            with ExitStack() as es:
                wo = c.sb(es, "e4w", [128, 8, 1024], F32R)
                c.dma(wo[:], e_wout[:, :].re("(kc p) f -> p kc f", p=128), q="gpsimd")
                mr = Ring([c.sb(es, "e4m%d" % i, [128, 8, 512], F32R) for i in range(2)])
                xr_ = Ring([c.sb(es, "e4x%d" % i, [128, 8, 512]) for i in range(2)])
                orr = Ring([c.sb(es, "e4o%d" % i, [128, 8, 512]) for i in range(2)])
                for i in range(NT):
                    mt = mr.next()
                    xt = xr_.next()
                    c.dma(mt[:], fm(mixT, i, 8), q="gpsimd")
                    c.dma(xt[:], fm(xT, i, 8))
                    o = orr.next()
                    for dc in range(8):
                        pb = P[dc % 4]
                        for kc in range(8):
                            c.mm(pb[:, :], wo[:, kc, dc * 128:(dc + 1) * 128], mt[:, kc, :], start=(kc == 0), stop=(kc == 7))
                        c.stt(o[:, dc, :], pb[:, :], mod[:, 16 + dc:17 + dc], xt[:, dc, :], ALU.mult, ALU.add)
                    c.dma(fm(x1T, i, 8), o[:])
                c.barrier()

        def moe_phase(l, xin, xout):
            with ExitStack() as es:
                st = mk_norm(es, "mn")
                xr_ = Ring([c.sb(es, "mx%d" % i, [128, 8, 512]) for i in range(1)])
                hb = c.sb(es, "mh", [128, 8, 512], F32R)
                hf = c.sb(es, "mhf", [128, 8, 512])
                hr = c.sb(es, "mhr", [128, 8, 512], F32R)
                rw = c.sb(es, "mrw", [128, 8, NE])
                rb = c.sb(es, "mrb", [1, NE])
                bg = c.sb(es, "mbg", [128, NE, 16])
                bg2 = c.sb(es, "mbg2", [128, NE, 8])
                bd = c.sb(es, "mbd", [NE, D])
                sel = c.sb(es, "msel", [NE, NE, 128])
                c.dma(rw[:], router_w[l].re("(kc p) e -> p kc e", p=128))
                c.dma(rb[:], router_b[l:l + 1, :])
                c.dma(bg[:], bgu[l])
                c.dma(bd[:], bdn[l])
                c.dma(sel[:], sel_d[:])
                c.ts(bg2[:, :, :], bg[:, :, 0:8], 1.702)
                lg = c.sb(es, "mlg", [128, NE])
                mx8 = c.sb(es, "mmx8", [128, 8])
                nmx = c.sb(es, "mnmx", [128, 1])
                msk = c.sb(es, "mmsk", [128, NE])
                ex = c.sb(es, "mex", [128, NE])
                ssum = c.sb(es, "mss", [128, 1])
                gts = c.sb(es, "mgts", [128, NE])
                gT = c.sb(es, "mgT", [NE, 512])
                gbc = Ring([c.sb(es, "mgbc%d" % i, [128, 512]) for i in range(2)])
                wgr = Ring([c.sb(es, "mwg%d" % i, [128, 8, 256], F32R) for i in range(3)])
                wdr = Ring([c.sb(es, "mwd%d" % i, [128, 1024], F32R) for i in range(12)])
                silr = Ring([c.sb(es, "msil%d" % i, [128, 512]) for i in range(2)])
                l1r = Ring([c.sb(es, "ml1%d" % i, [128, 512]) for i in range(2)])
                c7 = 7.0 / (1.0 + np.exp(-1.702 * 7.0)) * 1.702
                for i in range(NT):
                    xt = xr_.next()
                    yacc = xt
                    c.dma(xt[:], fm(xin, i, 8))
                    norm_tile(st, xt, hf, gg[:, l * 2 + 1, :], mod[:, l * 48 + 24:l * 48 + 32], P[7])
                    for kc in range(8):
                        c.copy(hr[:, kc, :], hf[:, kc, :], eng="scalar")
                    for s in range(4):
                        for kc in range(8):
                            c.mm(P[6][:, 0:NE], hf[:, kc, s * 128:(s + 1) * 128], rw[:, kc, :], start=(kc == 0), stop=False)
                        c.mm(P[6][:, 0:NE], ones_f[0:1, :], rb[0:1, :], start=False, stop=True)
                        c.copy(lg[:, :], P[6][:, 0:NE])
                        c.max8(mx8[:, :], lg[:, :])
                        c.ts(nmx[:, :], mx8[:, 0:1], -1.0)
                        c.ts(msk[:, :], lg[:, :], mx8[:, 3:4], None, ALU.is_ge)
                        c.act(ex[:, :], lg[:, :], AF.Exp, bias=nmx[:, 0:1])
                        c.tt(ex[:, :], ex[:, :], msk[:, :], ALU.mult)
                        c.rsum(ssum[:, :], ex[:, :])
                        c.recip(ssum[:, :], ssum[:, :])
                        c.ts(gts[:, :], ex[:, :], ssum[:, 0:1])
                        c.transpose(P[6][0:NE, 128:256], gts[:, :], ident[:, :])
                        c.copy(gT[:, s * 128:(s + 1) * 128], P[6][0:NE, 128:256])
                    for dc in range(8):
                        pb = P[4 + (dc % 2)]
                        c.mm(pb[:, :], bd[:, dc * 128:(dc + 1) * 128], gT[:, :])
                        c.copy(yacc[:, dc, :], pb[:, :], eng=("scalar" if dc % 2 else "vector"))
                    for e in range(NE):
                        gb = gbc.next()
                        c.mm(P[6][:, :], sel[:, e, :], gT[:, :])
                        c.act(gb[:, :], P[6][:, :], AF.Identity, scale=1.0 / 1.702)
                        wds = []
                        for fc in range(8):
                            wg = wgr.next()
                            c.dma(wg[:], wgu[l, e, fc], q="gpsimd")
                            wd = wdr.next()
                            c.dma(wd[:], wdn[l, e, fc * 128:(fc + 1) * 128, :], q="gpsimd", max_dma_last_dim=4096)
                            wds.append(wd)
                            pg, pl = P[0 + 2 * (fc % 2)], P[1 + 2 * (fc % 2)]
                            for kc in range(8):
                                c.mm(pg[:, :], wg[:, kc, 0:128], hr[:, kc, :], start=(kc == 0), stop=(kc == 7))
                            for kc in range(8):
                                c.mm(pl[:, :], wg[:, kc, 128:256], hr[:, kc, :], start=(kc == 0), stop=(kc == 7))
                            sil = silr.next()
                            l1 = l1r.next()
                            c.act(sil[:, :], pg[:, :], AF.Silu, bias=bg2[:, e, fc:fc + 1], scale=1.702)
                            c.ts(l1[:, :], pl[:, :], bg[:, e, 8 + fc:9 + fc], 7.0, ALU.add, ALU.min)
                            c.ts(l1[:, :], l1[:, :], -7.0, 1.0, ALU.max, ALU.add)
                            c.stt(sil[:, :], sil[:, :], c7, l1[:, :], ALU.min, ALU.mult)
                            c.tt(hb[:, fc, :], sil[:, :], gb[:, :], ALU.mult)
                        for dc in range(8):
                            pb = P[4 + (dc % 2)]
                            for fc in range(8):
                                c.mm(pb[:, :], wds[fc][:, dc * 128:(dc + 1) * 128], hb[:, fc, :], start=(fc == 0), stop=(fc == 7))
                            c.tt(yacc[:, dc, :], yacc[:, dc, :], pb[:, :], ALU.add)
                    c.dma(hf[:], fm(xin, i, 8))
                    for dc in range(8):
                        c.stt(hf[:, dc, :], yacc[:, dc, :], mod[:, l * 48 + 40 + dc:l * 48 + 41 + dc], hf[:, dc, :],
                              ALU.mult, ALU.add)
                    c.dma(fm(xout, i, 8), hf[:])
                c.barrier()


        def moe_sparse(l, xin, xout):
            wgu_rows = wgu[:, :]
            wdn_rows = wdn[:, :]
            bgu_rows = View(bgu2, bgu2.t.rearrange("l r c -> (l r) c"))
            bdn_rows = View(bdn2, bdn2.t.rearrange("l r c -> (l r) c"))
            with ExitStack() as esO:
                wixu = c.sb(esO, "q_wixu", [128, NBLK, 4], U32)
                wdxu = c.sb(esO, "q_wdxu", [128, NBLK, 2], U32)
                bixu = c.sb(esO, "q_bixu", [128, NBLK], U32)
                dallu = c.sb(esO, "q_dallu", [128, NSUB, 4], U32)
                beu = c.sb(esO, "q_beu", [128, NBLK], U32)
                with ExitStack() as es:
                    st = mk_norm(es, "qn")
                    xt = c.sb(es, "q_x", [128, 8, 512])
                    hf = c.sb(es, "q_hf", [128, 8, 512])
                    rw = c.sb(es, "q_rw", [128, 8, NE])
                    rb = c.sb(es, "q_rb", [1, NE])
                    us = c.sb(es, "q_us", [128, 128])
                    eio = c.sb(es, "q_eio", [128, NE])
                    jv = c.sb(es, "q_jv", [128, NBLK])
                    tk = c.sb(es, "q_tk", [128, NSUB])
                    wid = c.sb(es, "q_wid", [128, 9])
                    c.dma(rw[:], router_w[l].re("(kc p) e -> p kc e", p=128))
                    c.dma(rb[:], router_b[l:l + 1, :])
                    c.dma(us[:], ustrict_d[:])
                    c.dma(eio[:], eiota_d[:])
                    c.dma(jv[:], jvec_d[:])
                    c.dma(tk[:], tokid_d[:])
                    c.dma(wid[:], widx_d[:])
                    ri0 = c.sb(es, "q_ri0", [128, NBLK * 4, 2])
                    c.dma(ri0[:], rowinit_d[:])
                    c.dma(rowinfo[:, :].re("(p a) c -> p a c", p=128), ri0[:])
                    zt = c.sb(es, "q_zt", [128, D])
                    c.memset(zt[:], 0.0)
                    for a in range(4):
                        c.dma(hrows[S + a * 128:S + (a + 1) * 128, :], zt[:])
                    macc = c.sb(es, "q_macc", [128, NSUB + 1, NE])
                    c.memset(macc[:, 0, :], 0.0)
                    tmpr = [Ring([c.sb(es, "q_tmp%d_%d" % (k, i), shp, dt_) for i in range(2)])
                            for k, (shp, dt_) in enumerate([([128, 4, NE], F32), ([128, 4, 8], F32), ([128, 4, 8], U32),
                                                            ([128, 4], F32), ([128, 4, NE], F32), ([128, 4, 4], F32),
                                                            ([128, 4], F32)])]
                    cumall = c.sb(es, "q_cum", [128, NSUB, NE])
                    idxf = c.sb(es, "q_idxf", [128, NSUB, 4])
                    gk = c.sb(es, "q_gk", [128, NSUB, 4])
                    hrr = Ring([c.sb(es, "q_hr%d" % i, [128, D]) for i in range(2)])
                    lg = c.sb(es, "q_lg", [128, NE])
                    mx8 = c.sb(es, "q_mx8", [128, 8])
                    ix8 = c.sb(es, "q_ix8", [128, 8], U32)
                    nmx = c.sb(es, "q_nmx", [128, 1])
                    msk = c.sb(es, "q_msk", [128, NE])
                    ex4 = c.sb(es, "q_ex4", [128, 4])
                    s4 = c.sb(es, "q_s4", [128, 1])
                    for i in range(NT):
                        c.dma(xt[:], fm(xin, i, 8))
                        norm_tile(st, xt, hf, gg[:, l * 2 + 1, :], mod[:, l * 48 + 24:l * 48 + 32], P[7])
                        lga, mxa, ixa, nma, mka, exa, s4a = [r.next() for r in tmpr]
                        for s_ in range(4):
                            g = 4 * i + s_
                            tsl = slice(s_ * 128, (s_ + 1) * 128)
                            for kc in range(8):
                                c.transpose(P[kc // 4][:, (kc % 4) * 128:(kc % 4 + 1) * 128], hf[:, kc, tsl], ident[:, :])
                            hr_ = hrr.next()
                            c.copy(hr_[:, 0:512], P[0][:, :], eng="scalar")
                            c.copy(hr_[:, 512:1024], P[1][:, :])
                            c.dma(hrows[g * 128:(g + 1) * 128, :], hr_[:])
                            for kc in range(8):
                                c.mm(P[6][:, s_ * NE:(s_ + 1) * NE], hf[:, kc, tsl], rw[:, kc, :], start=(kc == 0), stop=False)
                            c.mm(P[6][:, s_ * NE:(s_ + 1) * NE], ones_f[0:1, :], rb[0:1, :], start=False, stop=True)
                        c.copy(lga[:, :, :], P[6][:, 0:4 * NE].re("p (q e) -> p q e", q=4))
                        for s_ in range(4):
                            c.max8(mxa[:, s_, :], lga[:, s_, :])
                        for s_ in range(4):
                            c.max_index(ixa[:, s_, :], mxa[:, s_, :], lga[:, s_, :])
                        c.copy(idxf[:, 4 * i:4 * i + 4, :], ixa[:, :, 0:4])
                        c.ts(nma[:, :], mxa[:, :, 0], -1.0)
                        for s_ in range(4):
                            c.ts(mka[:, s_, :], lga[:, s_, :], mxa[:, s_, 3:4], None, ALU.is_ge)
                        for s_ in range(4):
                            c.act(exa[:, s_, :], mxa[:, s_, 0:4], AF.Exp, bias=nma[:, s_:s_ + 1])
                        for s_ in range(4):
                            g = 4 * i + s_
                            c.mm(P[5][:, s_ * NE:(s_ + 1) * NE], us[:, :], mka[:, s_, :], start=True, stop=False)
                            c.mm(P[5][:, s_ * NE:(s_ + 1) * NE], ones_f[:, :], macc[:, g, :], start=False, stop=True)
                            c.tt(macc[:, g + 1, :], macc[:, g, :], mka[:, s_, :], ALU.add)
                        for s_ in range(4):
                            c.rsum(s4a[:, s_:s_ + 1], exa[:, s_, :])
                        c.recip(s4a[:, :], s4a[:, :])
                        for s_ in range(4):
                            c.ts(gk[:, 4 * i + s_, :], exa[:, s_, :], s4a[:, s_:s_ + 1])
                        c.copy(cumall[:, 4 * i:4 * i + 4, :], P[5][:, 0:4 * NE].re("p (q e) -> p q e", q=4), eng="scalar")
                    cnt = c.sb(es, "q_cnt", [128, NE])
                    qi_ = c.sb(es, "q_qi", [128, NE], I32)
                    pad = c.sb(es, "q_pad", [128, NE])
                    incl = c.sb(es, "q_incl", [128, NE])
                    pstart = c.sb(es, "q_pstart", [128, NE])
                    one32 = c.sb(es, "q_one32", [128, NE])
                    c.memset(one32[:], 1.0)
                    c.mm(P[5][:, 0:NE], ones_f[:, :], macc[:, NSUB, :])
                    c.ts(cnt[:, :], P[5][:, 0:NE], 511.0, 1.0 / 512, ALU.add, ALU.mult)
                    c.ts(cnt[:, :], cnt[:, :], -0.5 + 1.0 / 1024, None, ALU.add)
                    c.copy(qi_[:, :], cnt[:, :])
                    c.copy(pad[:, :], qi_[:, :])
                    c.scan(incl[:, :], one32[:, :], pad[:, :])
                    c.tt(pstart[:, :], incl[:, :], pad[:, :], ALU.subtract)
                    c.ts(pstart[:, :], pstart[:, :], 512.0)
                    bef = c.sb(es, "q_bef", [128, NBLK])
                    c.memset(bef[:], 0.0)
                    for e in range(NE):
                        c.stt(bef[:, :], jv[:, :], incl[:, e:e + 1], bef[:, :], ALU.is_ge, ALU.add)
                    c.ts(bef[:, :], bef[:, :], float(NE - 1), None, ALU.min)
                    if l > 0:
                        befl = c.sb(es, "q_befl", [128, NBLK])
                        c.ts(befl[:, :], bef[:, :], float(l * NE), None, ALU.add)
                        c.copy(beu[:, :], befl[:, :])
                    else:
                        c.copy(beu[:, :], bef[:, :])
                    wixf = c.sb(es, "q_wixf", [128, NBLK, 4])
                    for g_ in range(4):
                        c.ts(wixf[:, :, g_], bef[:, :], 512.0, wid[:, g_:g_ + 1], ALU.mult, ALU.add)
                    if l > 0:
                        c.ts(wixf[:, :, :], wixf[:, :, :], float(l * NE * 512), None, ALU.add)
                    c.copy(wixu[:, :, :], wixf[:, :, :])
                    for h_ in range(2):
                        c.ts(wixf[:, :, h_], bef[:, :], 256.0, wid[:, h_:h_ + 1], ALU.mult, ALU.add)
                    if l > 0:
                        c.ts(wixf[:, :, 0:2], wixf[:, :, 0:2], float(l * NE * 256), None, ALU.add)
                    c.copy(wdxu[:, :, :], wixf[:, :, 0:2])
                    c.ts(bef[:, :], bef[:, :], 128.0, wid[:, 8:9], ALU.mult, ALU.add)
                    if l > 0:
                        c.ts(bef[:, :], bef[:, :], float(l * NE * 128), None, ALU.add)
                    c.copy(bixu[:, :], bef[:, :])
                    dall = c.sb(es, "q_dall", [128, NSUB, 4])
                    df = c.sb(es, "q_df", [128, NE])
                    oh = c.sb(es, "q_oh", [128, NE])
                    for g in range(NSUB):
                        c.tt(df[:, :], pstart[:, :], cumall[:, g, :], ALU.add)
                        for k in range(4):
                            c.ts(oh[:, :], eio[:, :], idxf[:, g, k:k + 1], None, ALU.is_equal)
                            c.tt(oh[:, :], oh[:, :], df[:, :], ALU.mult)
                            c.rsum(dall[:, g, k:k + 1], oh[:, :])
                    c.copy(dallu[:, :, :], dall[:, :, :])
                    sa = c.sb(es, "q_sa", [128, NSUB, 4])
                    sai = c.sb(es, "q_sai", [128, NSUB, 4], I32)
                    sp_ = c.sb(es, "q_sp", [128, NSUB, 4])
                    sidxu = c.sb(es, "q_sidxu", [128, NSUB, 4], U32)
                    c.ts(sa[:, :, :], dall[:, :, :], 1.0 / 128, -0.5 + 1.0 / 256, ALU.mult, ALU.add)
                    c.copy(sai[:, :, :], sa[:, :, :])
                    c.copy(sa[:, :, :], sai[:, :, :])
                    c.stt(sp_[:, :, :], sa[:, :, :], -128.0, dall[:, :, :], ALU.mult, ALU.add)
                    c.stt(sp_[:, :, :], sp_[:, :, :], float(NBLK * 4), sa[:, :, :], ALU.mult, ALU.add)
                    c.copy(sidxu[:, :, :], sp_[:, :, :])
                    pay = c.sb(es, "q_pay", [128, NSUB, 4, 2])
                    for k in range(4):
                        c.copy(pay[:, :, k, 0], tk[:, :])
                    c.copy(pay[:, :, :, 1], gk[:, :, :])
                    PQ = c.engs["gpsimd"]
                    for g in range(NSUB):
                        for k in range(4):
                            c.scatter(rowinfo[:, :], pay[:, g, k, :], sidxu[:, g, k:k + 1])
                        if g % 8 == 7:
                            d_ = pay.dsem
                            PQ["h"].wait_ge(d_["sem"], d_["cnt"])
                            PQ["waited"][d_["key"]] = d_["cnt"]
                    c.barrier()
                with ExitStack() as es:
                    rir = Ring([c.sb(es, "b_ri%d" % i, [128, 4, 2]) for i in range(3)])
                    riur = Ring([c.sb(es, "b_riu%d" % i, [128, 4], U32) for i in range(3)])
                    xgr = Ring([c.sb(es, "b_xg%d" % i, [128, 4, D], BF16) for i in range(3)])
                    identb = c.sb(es, "b_identb", [128, 128], BF16)
                    c.copy(identb[:, :], ident[:, :])
                    xbT = c.sb(es, "b_xbT", [128, 8, 512], BF16)
                    at = c.sb(es, "b_a", [128, 8, 512], BF16)
                    yrow = c.sb(es, "b_yrow", [128, 4, D])
                    bbr = Ring([c.sb(es, "b_bb%d" % i, [128, D]) for i in range(3)])
                    bgr = Ring([c.sb(es, "b_bg%d" % i, [128, 16]) for i in range(3)])
                    bg2r = Ring([c.sb(es, "b_bg2%d" % i, [128, 8]) for i in range(3)])
                    bdr = Ring([c.sb(es, "b_bd%d" % i, [128, 8]) for i in range(3)])
                    stgr = Ring([c.sb(es, "b_stg%d" % i, [128, 4096]) for i in range(3)])
                    wgr = Ring([c.sb(es, "b_wg%d" % i, [128, 8, 256], BF16) for i in range(6)])
                    wdr = Ring([c.sb(es, "b_wd%d" % i, [128, 1024], BF16) for i in range(10)])
                    silr = Ring([c.sb(es, "b_sil%d" % i, [128, 512]) for i in range(2)])
                    l1r = Ring([c.sb(es, "b_l1%d" % i, [128, 512]) for i in range(2)])
                    c7 = 7.0 / (1.0 + np.exp(-1.702 * 7.0)) * 1.702
                    riall = c.sb(es, "b_riall", [128, NBLK * 4, 2])
                    riuall = c.sb(es, "b_riuall", [128, NBLK * 4], U32)
                    c.dma(riall[:], rowinfo[:, :].re("(p a) c -> p a c", p=128))
                    c.copy(riuall[:, :], riall[:, :, 0])

                    def prefetch(jn):
                        xg_ = xgr.next()
                        for a in range(4):
                            c.gather(xg_[:, a, :], hrows[:, :], riuall[:, jn * 4 + a:jn * 4 + a + 1])
                        bgt_, bg2_ = bgr.next(), bg2r.next()
                        c.gather(bgt_[:, :], bgu_rows, bixu[:, jn:jn + 1])
                        c.ts(bg2_[:, :], bgt_[:, 0:8], 1.702)
                        bb_ = bbr.next()
                        c.gather(bb_[:, :], bdnr_d[:, :], beu[:, jn:jn + 1])
                        return xg_, jn, bgt_, bg2_, bb_

                    def job_g(jn, g_, wgs):
                        stg = stgr.next()
                        c.gather(stg[:, :], wgu_rows, wixu[:, jn, g_:g_ + 1])
                        for fl in range(2):
                            wg = wgr.next()
                            c.copy(wg[:, :, :].re("p kc j -> p (kc j)"), stg[:, fl * 2048:(fl + 1) * 2048], eng="scalar")
                            wgs[2 * g_ + fl] = wg

                    def job_d(jn, h_, wds):
                        stg = stgr.next()
                        c.gather(stg[:, :], wdn_rows, wdxu[:, jn, h_:h_ + 1])
                        for fl in range(4):
                            wd = wdr.next()
                            c.copy(wd[:, :], stg[:, fl * 1024:(fl + 1) * 1024])
                            wds[4 * h_ + fl] = wd

                    def trans_in(xg_):
                        for kc in range(8):
                            pb = P[6 + (kc % 2)]
                            pbv = pb[:, :].bc(BF16)
                            for a in range(4):
                                c.transpose(pbv[:, a * 128:(a + 1) * 128], xg_[:, a, kc * 128:(kc + 1) * 128], identb[:, :])
                            c.copy(xbT[:, kc, :], pbv[:, 0:512], eng=("scalar" if kc % 2 else "vector"))

                    def stage_b(fc, wg, bgt, bg2):
                        pg, pl = P[0 + 2 * (fc % 2)], P[1 + 2 * (fc % 2)]
                        for kc in range(8):
                            c.mm(pg[:, :], wg[:, kc, 0:128], xbT[:, kc, :], start=(kc == 0), stop=(kc == 7))
                        for kc in range(8):
                            c.mm(pl[:, :], wg[:, kc, 128:256], xbT[:, kc, :], start=(kc == 0), stop=(kc == 7))
                        sil = silr.next()
                        l1 = l1r.next()
                        c.act(sil[:, :], pg[:, :], AF.Silu, bias=bg2[:, fc:fc + 1], scale=1.702)
                        c.ts(l1[:, :], pl[:, :], bgt[:, 8 + fc:9 + fc], 7.0, ALU.add, ALU.min)
                        c.ts(l1[:, :], l1[:, :], -7.0, 1.0, ALU.max, ALU.add)
                        c.stt(sil[:, :], sil[:, :], c7, l1[:, :], ALU.min, ALU.mult)
                        c.ts(at[:, fc, :], sil[:, :], 1.0 / 1.702)

                    cur = prefetch(0)
                    trans_in(cur[0])
                    wgs, wds = [None] * 8, [None] * 8
                    job_g(0, 0, wgs)
                    job_g(0, 1, wgs)
                    for j in range(NBLK):
                        xg, ri, bgt, bg2, bdt = cur
                        if j + 1 < NBLK:
                            nxt = prefetch(j + 1)
                        stage_b(0, wgs[0], bgt, bg2)
                        stage_b(1, wgs[1], bgt, bg2)
                        job_d(j, 0, wds)
                        job_g(j, 2, wgs)
                        stage_b(2, wgs[2], bgt, bg2)
                        stage_b(3, wgs[3], bgt, bg2)
                        job_g(j, 3, wgs)
                        job_d(j, 1, wds)
                        stage_b(4, wgs[4], bgt, bg2)
                        stage_b(5, wgs[5], bgt, bg2)
                        stage_b(6, wgs[6], bgt, bg2)
                        stage_b(7, wgs[7], bgt, bg2)
                        wds_j = list(wds)
                        if j + 1 < NBLK:
                            trans_in(nxt[0])
                            wgs = [None] * 8
                            job_g(j + 1, 0, wgs)
                            job_g(j + 1, 1, wgs)
                        for a in range(4):
                            for hf_ in range(2):
                                pb = P[4 + ((2 * a + hf_) % 2)]
                                c.mm(pb[:, :], ones_f[0:1, :], bdt[0:1, hf_ * 512:(hf_ + 1) * 512], start=True, stop=False)
                                for fc in range(8):
                                    c.mm(pb[:, :], at[:, fc, a * 128:(a + 1) * 128], wds_j[fc][:, hf_ * 512:(hf_ + 1) * 512],
                                         start=False, stop=(fc == 7))
                                if hf_ == 0:
                                    c.ts(yrow[:, a, 0:512], pb[:, :], riall[:, ri * 4 + a, 1:2])
                                else:
                                    c.act(yrow[:, a, 512:1024], pb[:, :], AF.Identity, scale=riall[:, ri * 4 + a, 1:2])
                        c.dma(yrows[j * 512:(j + 1) * 512, :].re("(a p) d -> p a d", p=128), yrow[:])
                        if j + 1 < NBLK:
                            cur = nxt
                    c.barrier()
                with ExitStack() as es:
                    xt = c.sb(es, "c_x", [128, 8, 512])
                    ykr = Ring([c.sb(es, "c_yk%d" % i, [128, 4, D]) for i in range(4)])
                    tsumr = Ring([c.sb(es, "c_ts%d" % i, [128, D]) for i in range(2)])
                    for i in range(NT):
                        c.dma(xt[:], fm(xin, i, 8))
                        for s_ in range(4):
                            g = 4 * i + s_
                            yk = ykr.next()
                            tsum = tsumr.next()
                            for k in range(4):
                                c.gather(yk[:, k, :], yrows[:, :], dallu[:, g, k:k + 1])
                            c.tt(tsum[:, :], yk[:, 0, :], yk[:, 1, :], ALU.add)
                            c.tt(tsum[:, :], tsum[:, :], yk[:, 2, :], ALU.add)
                            c.tt(tsum[:, :], tsum[:, :], yk[:, 3, :], ALU.add)
                            for kc in range(8):
                                c.transpose(P[kc // 4][:, (kc % 4) * 128:(kc % 4 + 1) * 128],
                                            tsum[:, kc * 128:(kc + 1) * 128], ident[:, :])
                            for kc in range(8):
                                c.stt(xt[:, kc, s_ * 128:(s_ + 1) * 128], P[kc // 4][:, (kc % 4) * 128:(kc % 4 + 1) * 128],
                                      mod[:, l * 48 + 40 + kc:l * 48 + 41 + kc], xt[:, kc, s_ * 128:(s_ + 1) * 128],
                                      ALU.mult, ALU.add)
                        c.dma(fm(xout, i, 8), xt[:])
                    c.barrier()

        moe_fn = moe_sparse if moe_mode == "sparse" else moe_phase

        if "moe0" in phases:
            moe_fn(0, (xT if moe_src == 'xT' else x1T), x2T)

        if "odd" in phases:
            with ExitStack() as es:
                win = c.sb(es, "o_win_sb", [128, 8, 1536], F32R)
                for kc in range(8):
                    c.dma(win[:, kc, :], o_win[kc * 128:(kc + 1) * 128, :], q="gpsimd")
                wo = c.sb(es, "o_wo", [128, 8, 1024], F32R)
                c.dma(wo[:], o_wout[:, :].re("(kc p) f -> p kc f", p=128), q="gpsimd")
                pw = c.sb(es, "o_pw", [128, 4, 128], F32R)
                c.dma(pw[:], poolw_d[:], q="gpsimd")
                psc = c.sb(es, "o_psc", [128, 4])
                c.dma(psc[:], pools_d[:])
                sgg = c.sb(es, "o_sgg", [128, 512])
                c.dma(sgg[:], sgug_d[:])
                sw = c.sb(es, "o_sw", [128, 4, 128], F32R)
                with ExitStack() as es2:
                    swf = c.sb(es2, "o_swf", [128, 4, 128])
                    c.dma(swf[:], sguw_d[:])
                    cm = c.sb(es2, "o_cm", [128, 128])
                    c.dma(cm[:], cmask_d[:, 0, 0:128])
                    for hh in range(4):
                        c.tt(sw[:, hh, :], swf[:, hh, :], cm[:, :], ALU.mult)
                    c.barrier()
                sbb = c.sb(es, "o_sb", [1, 512], F32R)
                c.dma(sbb[:], sgub_d[:], q="gpsimd")
                i16 = c.sb(es, "o_i16", [128, 4, 16])
                c.dma(i16[:], inv16_d[:])
                st = mk_norm(es, "on")
                xt_ = c.sb(es, "o_x", [128, 8, 512])
                h = c.sb(es, "o_h", [128, 8, 512], F32R)
                ub = c.sb(es, "o_ub", [128, 4, 528])
                s2 = c.sb(es, "o_s2", [128, 528])
                s3 = c.sb(es, "o_s3", [128, 528])
                mix = c.sb(es, "o_mix", [128, 8, 512], F32R)
                ug = c.sb(es, "o_ug", [128, 4, 512])
                zt4 = c.sb(es, "o_zt4", [128, 4, 512])
                g14 = c.sb(es, "o_g14", [128, 4, 512])
                vss4 = c.sb(es, "o_vss4", [128, 4])
                vt4 = c.sb(es, "o_vt4", [128, 4, 512], F32R)
                pl_ = c.sb(es, "o_pl", [128, 512], F32R)
                c.memset(ub[:], 0.0)
                wins = (2, 4, 8, 16)
                for i in range(NT):
                    c.dma(xt_[:], fm((xT if odd_src == 'xT' else x2T), i, 8))
                    norm_tile(st, xt_, h, gg[:, 2, :], mod[:, 48:56], P[7])
                    for gi in range(4):
                        pb = P[gi % 2]
                        for kc in range(8):
                            c.mm(pb[:, :], win[:, kc, gi * 128:(gi + 1) * 128], h[:, kc, :], start=(kc == 0), stop=(kc == 7))
                        c.copy(ub[:, gi, 16:528], pb[:, :], eng="scalar")
                        w = wins[gi]
                        cur = ub[:, gi, :]
                        k = 1
                        dst = s2
                        while k < w:
                            nxt = dst[:, :]
                            c.tt(nxt[:, 2 * k - 1:528], cur[:, 2 * k - 1:528], cur[:, k - 1:528 - k], ALU.add)
                            cur = nxt
                            dst = s3 if dst is s2 else s2
                            k *= 2
                        c.stt(pl_[:, :], cur[:, 16:528], 1.0 / w, ub[:, gi, 16:528], ALU.mult, ALU.subtract)
                        if i == 0:
                            c.tt(zt4[:, 0, 0:16], cur[:, 16:32], i16[:, gi, :], ALU.mult)
                            c.tt(pl_[:, 0:16], zt4[:, 0, 0:16], ub[:, gi, 16:32], ALU.subtract)
                        c.mm(P[2 + gi % 2][:, :], pw[:, gi, :], pl_[:, :])
                        c.ts(mix[:, gi, :], P[2 + gi % 2][:, :], psc[:, gi:gi + 1])
                        c.copy(ub[:, gi, 0:16], ub[:, gi, 512:528], eng="scalar")
                    for m in range(4):
                        for kc in range(8):
                            c.mm(P[m][:, :], win[:, kc, 512 + m * 128:512 + (m + 1) * 128], h[:, kc, :], start=(kc == 0), stop=(kc == 7))
                        c.copy(zt4[:, m, :], P[m][:, :], eng="scalar")
                    gelu_tanh_n(c, [ug[:, m, :] for m in range(4)], [zt4[:, m, :] for m in range(4)],
                                [g14[:, m, :] for m in range(4)], [g14[:, m, :] for m in range(4)])
                    for s_ in range(4):
                        ts_ = slice(s_ * 128, (s_ + 1) * 128)
                        for kc in range(8):
                            c.mm(P[4 + s_][:, :], h[:, kc, ts_], win[:, kc, 1024:1536], start=(kc == 0), stop=(kc == 7))
                        c.copy(zt4[:, s_, :], P[4 + s_][:, :], eng="scalar")
                    gelu_tanh_n(c, [g14[:, q, :] for q in range(4)], [zt4[:, q, :] for q in range(4)],
                                [g14[:, q, :] for q in range(4)], [g14[:, q, :] for q in range(4)])
                    for q in range(4):
                        c.tt(zt4[:, q, :], g14[:, q, :], g14[:, q, :], ALU.mult)
                    for q in range(4):
                        c.rsum(vss4[:, q:q + 1], zt4[:, q, :])
                    c.ts(vss4[:, :], vss4[:, :], 1.0 / 512, EPS, ALU.mult, ALU.add)
                    c.act(vss4[:, :], vss4[:, :], AF.Sqrt)
                    c.recip(vss4[:, :], vss4[:, :])
                    for q in range(4):
                        c.stt(vt4[:, q, :], g14[:, q, :], vss4[:, q:q + 1], sgg[:, :], ALU.mult, ALU.mult)
                    for q in range(4):
                        for hh in range(4):
                            c.mm(P[4 + q][:, hh * 128:(hh + 1) * 128], vt4[:, q, hh * 128:(hh + 1) * 128], sw[:, hh, :], start=True, stop=False)
                            c.mm(P[4 + q][:, hh * 128:(hh + 1) * 128], ones_r[0:1, :], sbb[0:1, hh * 128:(hh + 1) * 128], start=False, stop=True)
                    for q in range(4):
                        for hh in range(4):
                            c.tt(mix[:, 4 + hh, q * 128:(q + 1) * 128], ug[:, hh, q * 128:(q + 1) * 128], P[4 + q][:, hh * 128:(hh + 1) * 128], ALU.mult)
                    for dc in range(8):
                        pb = P[dc % 4]
                        for kc in range(8):
                            c.mm(pb[:, :], wo[:, kc, dc * 128:(dc + 1) * 128], mix[:, kc, :], start=(kc == 0), stop=(kc == 7))
                        c.stt(xt_[:, dc, :], pb[:, :], mod[:, 48 + 16 + dc:48 + 17 + dc], xt_[:, dc, :], ALU.mult, ALU.add)
                    c.dma(fm(x3T, i, 8), xt_[:])
                c.barrier()

        if "moe1" in phases:
            moe_fn(1, x3T, outT)
        c.barrier()
    return nc


def host_inputs(inp, b, S):
    f = np.float32
    g = lambda k: np.asarray(inp[k])
    chunkT = lambda v, n: np.ascontiguousarray(v.reshape(n, 128).T)
    m = {}
    m["xT"] = np.ascontiguousarray(g("x")[b, :S].T)
    m["cT"] = chunkT(g("c")[b], 8)
    m["pos"] = np.ascontiguousarray(g("positions")[b:b + 1, :S]).astype(np.int32)
    m["ada_w"] = g("ada_w")
    m["ada_bT"] = chunkT(g("ada_b").reshape(-1), 96)
    m["nmg"] = chunkT(g("norm_mix_g").reshape(-1), 16)
    m["nfg"] = chunkT(g("norm_ffn_g").reshape(-1), 16)
    m["router_w"] = g("router_w")
    m["router_b"] = g("router_b")
    wg = g("moe_w_gu")
    wg = wg.reshape(2, NE, 8, 128, 2, 8, 128)
    wg = wg.reshape(2, NE, 8, 128, 2, 4, 2, 128)
    m["wgu"] = np.ascontiguousarray(wg.transpose(0, 1, 5, 3, 6, 2, 4, 7)).reshape(2 * NE * 4 * 128, 4096)
    m["bgu"] = np.ascontiguousarray(g("moe_b_gu").reshape(2, NE, 16, 128).transpose(0, 3, 1, 2))
    m["bgu2"] = np.ascontiguousarray(g("moe_b_gu").reshape(2, NE, 16, 128).transpose(0, 1, 3, 2)).reshape(2, NE * 128, 16)
    m["bdnr"] = g("moe_b_dn").reshape(2 * NE, D)
    m["bdn2"] = np.ascontiguousarray(g("moe_b_dn").reshape(2, NE, 8, 128).transpose(0, 1, 3, 2)).reshape(2, NE * 128, 8)
    NSUB = S // 128
    NBLK = (4 * S + NE * 511 + 511) // 512
    pp = np.arange(128)
    m["ustrict"] = (pp[None, :] > pp[:, None]).astype(f)
    m["jvec"] = np.broadcast_to(np.arange(NBLK, dtype=f)[None, :], (128, NBLK))
    m["eiota"] = np.broadcast_to(np.arange(NE, dtype=f)[None, :], (128, NE))
    m["tokid"] = (np.arange(NSUB)[None, :] * 128 + pp[:, None]).astype(f)
    wi = np.zeros((128, 9), f)
    wi[:, 0:8] = np.arange(8)[None, :] * 128 + pp[:, None]
    wi[:, 8] = pp
    m["widx"] = wi
    rinit = np.zeros((128, NBLK * 4, 2), f)
    rinit[:, :, 0] = S + (np.arange(NBLK * 4)[None, :] % 4) * 128 + pp[:, None]
    m["rowinit"] = rinit
    wd_ = g("moe_w_dn").reshape(2, NE, 2, 4, 128, D)
    m["wdn"] = np.ascontiguousarray(wd_.transpose(0, 1, 2, 4, 3, 5)).reshape(2 * NE * 2 * 128, 4096)
    m["bdn"] = g("moe_b_dn")
    m["e_win"] = g("even_w_in")[0]
    m["qng"] = chunkT(g("mla_q_norm_g")[0], 2)
    m["kvng"] = chunkT(g("mla_kv_norm_g")[0], 1)
    wuq = g("mla_w_uq")[0]
    perm = np.concatenate([np.arange(64), np.arange(80, 96), np.arange(64, 80)])
    lay = lambda w: np.ascontiguousarray(w.reshape(2, 128, 8, 96).transpose(2, 1, 0, 3))
    m["wuq"] = lay(wuq)
    m["wuqs"] = lay(wuq[:, :, perm])
    wukv = g("mla_w_ukv")[0]
    m["wk"] = np.ascontiguousarray(wukv[:, :, 0:64].transpose(1, 0, 2))
    m["wv"] = np.ascontiguousarray(wukv[:, :, 64:128].transpose(1, 0, 2))
    qh = g("mla_q_head_g")[0]
    kh = g("mla_k_head_g")[0]
    m["qhg"] = np.ascontiguousarray(np.stack([qh, qh[perm]], 1))
    m["khg"] = np.ascontiguousarray(np.stack([kh, kh[perm]], 1))
    m["ident"] = np.eye(128, dtype=f)
    ipe = np.zeros((32, 2, 96), f)
    for j in range(32):
        ipe[j, 0, 64 + j] = 1.0
        ipe[(j + 16) % 32, 1, 64 + j] = 1.0
    m["ipe"] = ipe
    rc = np.zeros((96, 2), f)
    half = 16
    invf = (1.0 / (10000.0 ** (np.arange(half, dtype=np.float32) / half))).astype(f)
    rc[64:80, 0] = invf
    rc[80:96, 0] = invf
    rc[64:80, 1] = -1.0
    rc[80:96, 1] = 1.0
    m["ropec"] = rc
    sel = np.zeros((NE, NE, 128), f)
    for e in range(NE):
        sel[e, e, :] = 1.0
    m["sel"] = sel
    s65 = np.zeros((65, 64), f)
    s65[64, :] = 1.0
    m["sel65"] = s65
    cm = np.zeros((128, 4, 512), f)
    kk = np.arange(128)[:, None]
    qq = np.arange(512)[None, :]
    for j in range(4):
        cm[:, j, :] = (qq >= kk + 128 * j)
    m["cmask"] = cm
    i16 = np.zeros((128, 4, 16), f)
    for gi, w in enumerate((2, 4, 8, 16)):
        i16[:, gi, :] = 1.0 / np.minimum(np.arange(16) + 1, w)
    m["inv16"] = i16
    sm = lambda a: np.ascontiguousarray(a.reshape(16, 128, -1).transpose(1, 0, 2))
    a_re, a_im = g("s5_a_re")[0].reshape(-1), g("s5_a_im")[0].reshape(-1)
    ldt = np.repeat(g("s5_log_dt")[0], 64)
    m["s5p"] = np.ascontiguousarray(np.stack([chunkT(a_re, 16), chunkT(a_im, 16), chunkT(ldt, 16)], 1))
    br, bi = g("s5_b_re")[0].reshape(2048, 16), g("s5_b_im")[0].reshape(2048, 16)
    m["s5b"] = np.ascontiguousarray(np.stack([sm(br), sm(bi)], 1))
    cr = g("s5_c_re")[0].transpose(0, 2, 1).reshape(2048, 16)
    ci = g("s5_c_im")[0].transpose(0, 2, 1).reshape(2048, 16)
    m["s5c"] = np.ascontiguousarray(np.stack([sm(cr), sm(ci)], 1))
    m["s5d"] = chunkT(g("s5_d")[0], 4)
    m["gluw"] = g("s5_glu_w")[0]
    m["glub"] = chunkT(g("s5_glu_b")[0], 4)
    m["e_wout"] = g("even_w_out")[0]
    m["o_win"] = g("odd_w_in")[0]
    m["poolw"] = np.ascontiguousarray(g("pool_w")[0].transpose(1, 0, 2))
    m["pools"] = chunkT(g("pool_scale")[0], 4)
    m["sgug"] = np.ascontiguousarray(np.broadcast_to(g("sgu_norm_g")[0][None, :], (128, 512)))
    m["sguw"] = np.ascontiguousarray(g("sgu_w")[0].transpose(2, 0, 1))
    m["sgub"] = np.ascontiguousarray(g("sgu_b")[0].reshape(1, 512))
    m["o_wout"] = g("odd_w_out")[0]
    return {k: np.ascontiguousarray(v, dtype=(np.int32 if k == "pos" else f)) for k, v in m.items()}


def kernel(**inputs):
    B, S, _ = inputs["x"].shape
    nc = build(S)
    m0 = host_inputs(inputs, 0, S)
    maps = [m0]
    f = np.float32
    for b in range(1, B):
        mb = dict(m0)
        mb["xT"] = np.ascontiguousarray(np.asarray(inputs["x"])[b, :S].T, dtype=f)
        mb["cT"] = np.ascontiguousarray(np.asarray(inputs["c"])[b].reshape(8, 128).T, dtype=f)
        mb["pos"] = np.ascontiguousarray(np.asarray(inputs["positions"])[b:b + 1, :S]).astype(np.int32)
        maps.append(mb)
    res = run_bass_kernel_spmd(nc, maps, core_ids=list(range(B)))
    out = np.stack([np.ascontiguousarray(res.results[b]["outT"].T) for b in range(B)], 0)
    return out.astype(np.float32)
```

```python
import numpy as np
from contextlib import ExitStack
import concourse.bass as bass
import concourse.mybir as mybir
from concourse.bass_utils import run_bass_kernel_spmd

F32 = mybir.dt.float32
F32R = mybir.dt.float32r
I32 = mybir.dt.int32
BF16 = mybir.dt.bfloat16
U32 = mybir.dt.uint32
AF = mybir.ActivationFunctionType
ALU = mybir.AluOpType

D = 1024
EPS = 1e-6
NE = 32
TWO_PI = 2.0 * np.pi


class View:
    def __init__(self, buf, ap):
        self.buf = buf
        self.ap = ap

    def bc(self, dt):
        return View(self.buf, self.ap.bitcast(dt))

    def re(self, s, **kw):
        return View(self.buf, self.ap.rearrange(s, **kw))

    def __getitem__(self, k):
        return View(self.buf, self.ap[k])


class Buf:
    def __init__(self, t, name, dram=False):
        self.t = t
        self.name = name
        self.dram = dram
        self.lw = {}
        self.rd = {}
        self.dsem = None

    def __getitem__(self, k):
        return View(self, self.t[k])


class Ctx:
    def __init__(self, nc):
        self.nc = nc
        self.engs = {}
        for name in ["tensor", "vector", "scalar", "gpsimd", "sync"]:
            self.engs[name] = dict(h=getattr(nc, name), sem=nc.alloc_semaphore("s_" + name), cnt=0,
                                   waited={}, name=name)
        self.dpool = {}
        self.dall = []
        self.nd = 0
        self.phase_bufs = []
        self.nops = 0

    def sb(self, es, name, shape, dt=F32):
        self.nsb = getattr(self, "nsb", 0) + 1
        name = "%s_%d" % (name, self.nsb)
        t = es.enter_context(self.nc.sbuf_tensor(name, list(shape), dt))
        b = Buf(t, name)
        self.phase_bufs.append(b)
        return b

    def ps(self, es, name, shape, dt=F32):
        t = es.enter_context(self.nc.psum_tensor(name, list(shape), dt))
        b = Buf(t, name)
        return b

    def dram(self, name, shape, dt=F32, kind="Internal"):
        t = self.nc.dram_tensor(name, list(shape), dt, kind=kind)
        return Buf(t.ap(), name, dram=True)

    def _dsem(self, b, kind="hw"):
        if b.dsem is None:
            b.dsem = {}
        if kind not in b.dsem:
            pool = self.dpool.setdefault(kind, [])
            if pool:
                b.dsem[kind] = pool.pop()
            else:
                self.nd += 1
                d = dict(sem=self.nc.alloc_semaphore("d%d" % self.nd), cnt=0, key="d%d" % self.nd, kind=kind)
                self.dall.append(d)
                b.dsem[kind] = d
        return b.dsem[kind]

    def _wait(self, E, deps):
        for key, (sem, val) in deps.items():
            if key == "tensor" and E["name"] == "tensor":
                continue
            if E["waited"].get(key, 0) < val:
                E["h"].wait_ge(sem, val)
                E["waited"][key] = val

    @staticmethod
    def _merge(d, key, sem, val):
        if key not in d or d[key][1] < val:
            d[key] = (sem, val)

    def _deps(self, reads, writes):
        deps = {}
        for b in reads:
            for k, (s, v) in b.lw.items():
                self._merge(deps, k, s, v)
        for b in writes:
            for k, (s, v) in b.lw.items():
                self._merge(deps, k, s, v)
            for k, (s, v) in b.rd.items():
                self._merge(deps, k, s, v)
        return deps

    def _record(self, reads, writes, key, sem, val):
        for b in reads:
            self._merge(b.rd, key, sem, val)
        for b in writes:
            if b.dram:
                self._merge(b.lw, key, sem, val)
            else:
                b.lw = {key: (sem, val)}
                b.rd = {}

    def op(self, eng, fn, reads=(), writes=()):
        E = self.engs[eng]
        self._wait(E, self._deps(reads, writes))
        inst = fn(E["h"])
        E["cnt"] += 1
        inst.then_inc(E["sem"], 1)
        self._record(reads, writes, eng, E["sem"], E["cnt"])
        self.nops += 1
        return inst

    def dma(self, out, in_, q="sync", **kw):
        E = self.engs[q]
        self._wait(E, self._deps([in_.buf], [out.buf]))
        sb = in_.buf if out.buf.dram else out.buf
        d = self._dsem(sb, "sw" if q == "gpsimd" else "hw")
        inst = E["h"].dma_start(out=out.ap, in_=in_.ap, **kw)
        d["cnt"] += 16
        inst.then_inc(d["sem"], 16)
        self._record([in_.buf], [out.buf], d["key"], d["sem"], d["cnt"])
        self.nops += 1
        return inst

    def gather(self, out, src, offs):
        E = self.engs["gpsimd"]
        self._wait(E, self._deps([src.buf, offs.buf], [out.buf]))
        d = self._dsem(out.buf, "sw")
        inst = E["h"].indirect_dma_start(out=out.ap, out_offset=None, in_=src.ap,
                                         in_offset=bass.IndirectOffsetOnAxis(ap=offs.ap, axis=0))
        d["cnt"] += 16
        inst.then_inc(d["sem"], 16)
        self._record([src.buf, offs.buf], [out.buf], d["key"], d["sem"], d["cnt"])
        self.nops += 1
        return inst

    def scatter(self, dst, src, offs):
        E = self.engs["gpsimd"]
        self._wait(E, self._deps([src.buf, offs.buf], [dst.buf]))
        d = self._dsem(src.buf, "sw")
        inst = E["h"].indirect_dma_start(out=dst.ap, out_offset=bass.IndirectOffsetOnAxis(ap=offs.ap, axis=0),
                                         in_=src.ap, in_offset=None)
        d["cnt"] += 16
        inst.then_inc(d["sem"], 16)
        self._record([src.buf, offs.buf], [dst.buf], d["key"], d["sem"], d["cnt"])
        self.nops += 1
        return inst

    def barrier(self):
        deps = {}
        for n, X in self.engs.items():
            if X["cnt"] > 0:
                deps[n] = (X["sem"], X["cnt"])
        for d in self.dall:
            if d["cnt"] > 0:
                deps[d["key"]] = (d["sem"], d["cnt"])
        for n, E in self.engs.items():
            for key, (sem, val) in deps.items():
                if E["waited"].get(key, 0) < val:
                    E["h"].wait_ge(sem, val)
                    E["waited"][key] = val
        for b in self.phase_bufs:
            if b.dsem is not None:
                for kind, d in b.dsem.items():
                    self.dpool.setdefault(kind, []).append(d)
                b.dsem = None
        self.phase_bufs = []

    @staticmethod
    def _rb(*xs):
        return [x.buf for x in xs if isinstance(x, View)]

    @staticmethod
    def _a(x):
        return x.ap if isinstance(x, View) else x

    def mm(self, out, lhsT, rhs, start=True, stop=True):
        return self.op("tensor", lambda e: e.matmul(out.ap, lhsT.ap, rhs.ap, start=start, stop=stop),
                       [lhsT.buf, rhs.buf], [out.buf])

    def transpose(self, out, in_, ident):
        return self.op("tensor", lambda e: e.transpose(out.ap, in_.ap, ident.ap), [in_.buf, ident.buf], [out.buf])

    def act(self, out, in_, func, bias=None, scale=1.0, eng="scalar"):
        kw = {}
        if bias is not None:
            kw["bias"] = self._a(bias)
        kw["scale"] = self._a(scale)
        return self.op("scalar", lambda e: e.activation(out.ap, in_.ap, func, **kw),
                       self._rb(in_, bias, scale), [out.buf])

    def ts(self, out, in0, s1, s2=None, op0=ALU.mult, op1=None, eng="vector"):
        if op1 is None:
            f = lambda e: e.tensor_scalar(out.ap, in0.ap, self._a(s1), None, op0=op0)
        else:
            f = lambda e: e.tensor_scalar(out.ap, in0.ap, self._a(s1), self._a(s2), op0=op0, op1=op1)
        return self.op(eng, f, self._rb(in0, s1, s2), [out.buf])

    def tt(self, out, a, b, op, eng="vector"):
        return self.op(eng, lambda e: e.tensor_tensor(out.ap, a.ap, b.ap, op=op), [a.buf, b.buf], [out.buf])

    def stt(self, out, in0, scalar, in1, op0, op1):
        return self.op("vector", lambda e: e.scalar_tensor_tensor(out.ap, in0.ap, self._a(scalar), in1.ap,
                                                                  op0=op0, op1=op1),
                       self._rb(in0, scalar, in1), [out.buf])

    def copy(self, out, in_, eng="vector"):
        if eng == "scalar":
            return self.op("scalar", lambda e: e.activation(out.ap, in_.ap, AF.Identity), [in_.buf], [out.buf])
        return self.op(eng, lambda e: e.tensor_copy(out.ap, in_.ap), [in_.buf], [out.buf])

    def memset(self, out, val, eng="vector"):
        return self.op(eng, lambda e: e.memset(out.ap, val), [], [out.buf])

    def memset_r(self, out, val):
        self.memset(out.bc(F32), val)
        return self.copy(out, out.bc(F32))

    def recip(self, out, in_):
        return self.op("vector", lambda e: e.reciprocal(out.ap, in_.ap), [in_.buf], [out.buf])

    def scan(self, out, d0, d1, init=0.0):
        return self.op("vector", lambda e: e.tensor_tensor_scan(out.ap, d0.ap, d1.ap, self._a(init),
                                                                op0=ALU.mult, op1=ALU.add),
                       self._rb(d0, d1, init), [out.buf])

    def max_index(self, out, mx, vals):
        return self.op("vector", lambda e: e.max_index(out.ap, mx.ap, vals.ap), [mx.buf, vals.buf], [out.buf])

    def max8(self, out, in_):
        return self.op("vector", lambda e: e.max(out.ap, in_.ap), [in_.buf], [out.buf])

    def rsum(self, out, in_):
        return self.op("vector", lambda e: e.reduce_sum(out.ap, in_.ap, axis=mybir.AxisListType.X),
                       [in_.buf], [out.buf])


class Ring:
    def __init__(self, bufs):
        self.bufs = bufs
        self.i = 0

    def next(self):
        b = self.bufs[self.i % len(self.bufs)]
        self.i += 1
        return b


def range_reduce_sin(c, es, ang, out_sin, out_cos, shape, tagname):
    kf = c.sb(es, tagname + "_kf", shape)
    ki = c.sb(es, tagname + "_ki", shape, I32)
    r = c.sb(es, tagname + "_r", shape)
    m = c.sb(es, tagname + "_m", shape)
    sl = tuple(slice(None) for _ in shape)
    c.ts(kf[sl], ang, 1.0 / TWO_PI)
    c.copy(ki[sl], kf[sl])
    c.copy(kf[sl], ki[sl])
    c.stt(r[sl], kf[sl], -TWO_PI, ang, ALU.mult, ALU.add)

    def wrap(v):
        c.ts(m[sl], v, np.pi, TWO_PI, ALU.is_gt, ALU.mult)
        c.tt(v, v, m[sl], ALU.subtract)
        c.ts(m[sl], v, -np.pi, TWO_PI, ALU.is_lt, ALU.mult)
        c.tt(v, v, m[sl], ALU.add)

    wrap(r[sl])
    c.act(out_sin, r[sl], AF.Sin)
    c.ts(r[sl], r[sl], np.pi / 2, None, ALU.add)
    wrap(r[sl])
    c.act(out_cos, r[sl], AF.Sin)


def gelu_tanh(c, out, x, t1, t2):
    c.act(t1, x, AF.Square)
    c.ts(t1, t1, 0.044715, 1.0, ALU.mult, ALU.add)
    c.tt(t1, t1, x, ALU.mult)
    c.act(t2, t1, AF.Sigmoid, scale=1.5957691216057308)
    c.tt(out, x, t2, ALU.mult)


def gelu_tanh_n(c, outs, xs, t1s, t2s):
    n = len(outs)
    for i in range(n):
        c.act(t1s[i], xs[i], AF.Square)
    for i in range(n):
        c.ts(t1s[i], t1s[i], 0.044715, 1.0, ALU.mult, ALU.add)
    for i in range(n):
        c.tt(t1s[i], t1s[i], xs[i], ALU.mult)
    for i in range(n):
        c.act(t2s[i], t1s[i], AF.Sigmoid, scale=1.5957691216057308)
    for i in range(n):
        c.tt(outs[i], xs[i], t2s[i], ALU.mult)


def build(S, debug=False, phases=None, odd_src=None, moe_src=None, moe_mode="sparse"):
    NT = S // 512
    allph = ["ada", "e1", "attn", "s5", "e4", "moe0", "odd", "moe1"]
    phases = allph if phases is None else phases
    nc = bass.Bass("TRN2", target_bir_lowering=False)
    c = Ctx(nc)
    skind = "ExternalOutput" if debug else "Internal"

    def din(name, shape, dt=F32):
        return c.dram(name, shape, dt, kind="ExternalInput")

    def dscr(name, shape):
        return c.dram(name, shape, F32, kind=skind)

    xT = din("xT", [D, S])
    cT = din("cT", [128, 8])
    pos = din("pos", [1, S], I32)
    ada_w = din("ada_w", [2, D, 6 * D])
    ada_bT = din("ada_bT", [128, 96])
    nmg = din("nmg", [128, 16])
    nfg = din("nfg", [128, 16])
    router_w = din("router_w", [2, D, NE])
    router_b = din("router_b", [2, NE])
    wgu = din("wgu", [2 * NE * 4 * 128, 4096])
    bgu = din("bgu", [2, 128, NE, 16])
    wdn = din("wdn", [2 * NE * 2 * 128, 4096])
    bdn = din("bdn", [2, NE, D])
    e_win = din("e_win", [D, 928])
    qng = din("qng", [128, 2])
    kvng = din("kvng", [128, 1])
    wuq = din("wuq", [8, 128, 2, 96])
    wuqs = din("wuqs", [8, 128, 2, 96])
    wk = din("wk", [8, 128, 64])
    wv = din("wv", [8, 128, 64])
    qhg = din("qhg", [96, 2])
    khg = din("khg", [96, 2])
    ident_d = din("ident", [128, 128])
    ipe_d = din("ipe", [32, 2, 96])
    ropec = din("ropec", [96, 2])
    sel_d = din("sel", [32, NE, 128])
    sel65_d = din("sel65", [65, 64])
    cmask_d = din("cmask", [128, 4, 512])
    inv16_d = din("inv16", [128, 4, 16])
    s5p_d = din("s5p", [128, 3, 16])
    s5b_d = din("s5b", [128, 2, 16, 16])
    s5c_d = din("s5c", [128, 2, 16, 16])
    s5d_d = din("s5d", [128, 4])
    gluw_d = din("gluw", [512, 512])
    glub_d = din("glub", [128, 4])
    e_wout = din("e_wout", [D, D])
    o_win = din("o_win", [D, 1536])
    poolw_d = din("poolw", [128, 4, 128])
    pools_d = din("pools", [128, 4])
    sgug_d = din("sgug", [128, 512])
    sguw_d = din("sguw", [128, 4, 128])
    sgub_d = din("sgub", [1, 512])
    o_wout = din("o_wout", [D, D])
    outT = c.dram("outT", [D, S], F32, kind="ExternalOutput")
    NSUB = S // 128
    NBLK = (4 * S + NE * 511 + 511) // 512
    NROWS = NBLK * 512
    bgu2 = din("bgu2", [2, NE * 128, 16])
    bdn2 = din("bdn2", [2, NE * 128, 8])
    bdnr_d = din("bdnr", [2 * NE, D])
    ustrict_d = din("ustrict", [128, 128])
    jvec_d = din("jvec", [128, NBLK])
    eiota_d = din("eiota", [128, NE])
    tokid_d = din("tokid", [128, NSUB])
    widx_d = din("widx", [128, 9])
    rowinit_d = din("rowinit", [128, NBLK * 4, 2])
    hrows = dscr("hrows", [S + 512, D])
    rowinfo = dscr("rowinfo", [NROWS, 2])
    yrows = dscr("yrows", [NROWS, D])

    qnT = dscr("qnT", [256, S])
    kvnT = dscr("kvnT", [128, S])
    kpeT = dscr("kpeT", [32, S])
    uT = dscr("uT", [512, S])
    mixT = dscr("mixT", [D, S])
    x1T = dscr("x1T", [D, S])
    x2T = dscr("x2T", [D, S])
    x3T = dscr("x3T", [D, S])
    modD = dscr("modD", [128, 96])

    def fm(dv, i, nch):
        return dv[:, i * 512:(i + 1) * 512].re("(kc p) t -> p kc t", p=128)

    with ExitStack() as es0:
        P = [c.ps(es0, "P%d" % i, [128, 512]) for i in range(8)]
        ident = c.sb(es0, "ident_sb", [128, 128])
        ones_r = c.sb(es0, "ones_r", [128, 128], F32R)
        ones_f = c.sb(es0, "ones_f", [128, 128])
        mod = c.sb(es0, "mod", [128, 96])
        gg = c.sb(es0, "gg", [128, 4, 8])
        c.phase_bufs = []
        c.dma(ident[:], ident_d[:])
        c.memset_r(ones_r[:], 1.0)
        c.memset(ones_f[:], 1.0)

        if "ada" in phases:
            with ExitStack() as es:
                ct = c.sb(es, "ct", [128, 8])
                sig = c.sb(es, "sig", [128, 8])
                cact = c.sb(es, "cact", [128, 8, 2])
                adab = c.sb(es, "adab", [128, 96])
                nm = c.sb(es, "nm", [128, 16])
                nf = c.sb(es, "nf", [128, 16])
                wr = Ring([c.sb(es, "adaw%d" % i, [128, 8, 1024]) for i in range(2)])
                c.dma(ct[:], cT[:])
                c.dma(adab[:], ada_bT[:])
                c.dma(nm[:], nmg[:])
                c.dma(nf[:], nfg[:])
                c.act(sig[:], ct[:], AF.Sigmoid)
                c.tt(cact[:, :, 0], ct[:], sig[:], ALU.mult)
                c.tt(cact[:, :, 1], ct[:], sig[:], ALU.mult)
                for l in range(2):
                    for j in range(6):
                        wt = wr.next()
                        c.dma(wt[:], ada_w[l, :, j * 1024:(j + 1) * 1024].re("(kc p) f -> p kc f", p=128))
                        for fi in range(8):
                            idx = l * 48 + j * 8 + fi
                            for kc in range(8):
                                c.mm(P[0][:, idx * 2:idx * 2 + 2], wt[:, kc, fi * 128:(fi + 1) * 128],
                                     cact[:, kc, :], start=(kc == 0), stop=(kc == 7))
                c.tt(mod[:], P[0][:, 0:192].re("p (a b) -> p a b", b=2)[:, :, 0], adab[:], ALU.add)
                for l in range(2):
                    c.stt(gg[:, l * 2 + 0, :], mod[:, l * 48 + 8:l * 48 + 16], 1.0, nm[:, l * 8:(l + 1) * 8],
                          ALU.add, ALU.mult)
                    c.stt(gg[:, l * 2 + 1, :], mod[:, l * 48 + 32:l * 48 + 40], 1.0, nf[:, l * 8:(l + 1) * 8],
                          ALU.add, ALU.mult)
                if debug:
                    c.dma(modD[:], mod[:])
                c.barrier()

        def mk_norm(es, tag):
            sq = c.sb(es, tag + "_sq", [128, 8, 512], F32R)
            rs = c.sb(es, tag + "_rs", [128, 512])
            tr = Ring([c.sb(es, tag + "_t%d" % i, [128, 512]) for i in range(2)])
            return sq, rs, tr

        def rstd_from(rs, pv, n, npart=128):
            c.ts(rs[0:npart, :], pv, 1.0 / n, EPS, ALU.mult, ALU.add)
            c.act(rs[0:npart, :], rs[0:npart, :], AF.Ln)
            c.act(rs[0:npart, :], rs[0:npart, :], AF.Exp, scale=-0.5)

        def norm_tile(st, xt, hout, ggv, shv, pbank):
            sq, rs, tr = st
            for kc in range(8):
                c.act(sq[:, kc, :], xt[:, kc, :], AF.Square)
            for kc in range(8):
                c.mm(pbank[:, :], ones_r[:, :], sq[:, kc, :], start=(kc == 0), stop=(kc == 7))
            rstd_from(rs, pbank[:, :], float(D))
            for kc in range(8):
                t = tr.next()
                c.stt(t[:, :], xt[:, kc, :], ggv[:, kc:kc + 1], rs[:, :], ALU.mult, ALU.mult)
                c.act(hout[:, kc, :], t[:, :], AF.Identity, bias=shv[:, kc:kc + 1])

        if "e1" in phases:
            with ExitStack() as es:
                win = c.sb(es, "win", [128, 8, 928], F32R)
                c.dma(win[:], e_win[:, :].re("(kc p) f -> p kc f", p=128), q="gpsimd")
                qg = c.sb(es, "qg", [128, 2])
                kg = c.sb(es, "kg", [128, 1])
                c.dma(qg[:], qng[:])
                c.dma(kg[:], kvng[:])
                st = mk_norm(es, "n1")
                xr_ = Ring([c.sb(es, "e1x%d" % i, [128, 8, 512]) for i in range(2)])
                h = c.sb(es, "e1h", [128, 8, 512], F32R)
                sq2 = c.sb(es, "e1sq2", [128, 2, 512], F32R)
                rs2 = c.sb(es, "e1rs2", [128, 512])
                qn_sb = c.sb(es, "e1qn", [128, 2, 512])
                kvn_sb = c.sb(es, "e1kvn", [128, 512])
                kpe_sb = c.sb(es, "e1kpe", [32, 512])
                u_sb = c.sb(es, "e1u", [128, 4, 512])
                for i in range(NT):
                    xt = xr_.next()
                    c.dma(xt[:], fm(xT, i, 8))
                    norm_tile(st, xt, h, gg[:, 0, :], mod[:, 0:8], P[4])
                    for m in range(2):
                        for kc in range(8):
                            c.mm(P[m][:, :], win[:, kc, m * 128:(m + 1) * 128], h[:, kc, :], start=(kc == 0), stop=(kc == 7))
                    for kc in range(8):
                        c.mm(P[2][:, :], win[:, kc, 256:384], h[:, kc, :], start=(kc == 0), stop=(kc == 7))
                    for kc in range(8):
                        c.mm(P[3][:, :], win[:, kc, 384:512], h[:, kc, :], start=(kc == 0), stop=(kc == 7))
                    for m in range(2):
                        c.act(sq2[:, m, :], P[m][:, :], AF.Square)
                    for m in range(2):
                        c.mm(P[5][:, :], ones_r[:, :], sq2[:, m, :], start=(m == 0), stop=(m == 1))
                    rstd_from(rs2, P[5][:, :], 256.0)
                    for m in range(2):
                        c.stt(qn_sb[:, m, :], P[m][:, :], qg[:, m:m + 1], rs2[:, :], ALU.mult, ALU.mult)
                    c.dma(fm(qnT, i, 2), qn_sb[:])
                    c.act(sq2[:, 0, :], P[2][:, :], AF.Square)
                    c.mm(P[5][:, :], ones_r[:, :], sq2[:, 0, :])
                    rstd_from(rs2, P[5][:, :], 128.0)
                    c.stt(kvn_sb[:, :], P[2][:, :], kg[:, 0:1], rs2[:, :], ALU.mult, ALU.mult)
                    c.dma(kvnT[:, i * 512:(i + 1) * 512], kvn_sb[:])
                    c.copy(kpe_sb[:, :], P[3][0:32, :], eng="scalar")
                    c.dma(kpeT[:, i * 512:(i + 1) * 512], kpe_sb[:])
                    for m in range(4):
                        pb = P[6 + (m % 2)]
                        for kc in range(8):
                            c.mm(pb[:, :], win[:, kc, 416 + m * 128:416 + (m + 1) * 128], h[:, kc, :],
                                 start=(kc == 0), stop=(kc == 7))
                        c.copy(u_sb[:, m, :], pb[:, :], eng=("scalar" if m % 2 else "vector"))
                    c.dma(fm(uT, i, 4), u_sb[:])
                c.barrier()

        if "attn" in phases:
            with ExitStack() as es:
                cosT = c.sb(es, "cosT", [96, S])
                sinT = c.sb(es, "sinT", [96, S])
                with ExitStack() as es2:
                    posi = c.sb(es2, "posi", [96, S], I32)
                    ang = c.sb(es2, "ang", [96, S])
                    rc = c.sb(es2, "rc", [96, 2])
                    c.dma(rc[:], ropec[:])
                    c.dma(posi[:], View(pos, pos.t[0:1, :].partition_broadcast(96)))
                    c.copy(ang[:], posi[:])
                    c.ts(ang[:], ang[:], rc[:, 0:1])
                    range_reduce_sin(c, es2, ang[:], sinT[:], cosT[:], [96, S], "rr")
                    c.ts(sinT[:], sinT[:], rc[:, 1:2])
                    c.barrier()
                qn = c.sb(es, "a_qn", [128, 2, S], F32R)
                kvn = c.sb(es, "a_kvn", [128, S], F32R)
                kpe = c.sb(es, "a_kpe", [32, S], F32R)
                for m in range(2):
                    c.dma(qn[:, m, :], qnT[m * 128:(m + 1) * 128, :], q="gpsimd", max_dma_last_dim=4096)
                c.dma(kvn[:], kvnT[:], q="gpsimd", max_dma_last_dim=4096)
                c.dma(kpe[:], kpeT[:], q="gpsimd", max_dma_last_dim=4096)
                ipe = c.sb(es, "a_ipe", [32, 2, 96], F32R)
                c.dma(ipe[:], ipe_d[:], q="gpsimd")
                qh = c.sb(es, "a_qh", [96, 2])
                kh = c.sb(es, "a_kh", [96, 2])
                c.dma(qh[:], qhg[:])
                c.dma(kh[:], khg[:])
                cmask = c.sb(es, "a_cmask", [128, 4, 512])
                c.dma(cmask[:], cmask_d[:])
                sel65 = c.sb(es, "a_sel65", [65, 64])
                c.dma(sel65[:], sel65_d[:])
                wq = c.sb(es, "a_wq", [128, 2, 96], F32R)
                wqs = c.sb(es, "a_wqs", [128, 2, 96], F32R)
                wkt = c.sb(es, "a_wk", [128, 96], F32R)
                wvt = c.sb(es, "a_wv", [128, 64], F32R)
                qTt = c.sb(es, "a_qT", [96, S], F32R)
                kTt = c.sb(es, "a_kT", [96, S], F32R)
                V = c.sb(es, "a_V", [128, S // 128, 65], F32R)
                sq96 = c.sb(es, "a_sq96", [96, 512], F32R)
                rs96 = c.sb(es, "a_rs96", [128, 512])
                sq96b = c.sb(es, "a_sq96b", [96, 512], F32R)
                rs96b = c.sb(es, "a_rs96b", [128, 512])
                t1r = Ring([c.sb(es, "a_t1%d" % i, [96, 512]) for i in range(2)])
                t2r = Ring([c.sb(es, "a_t2%d" % i, [96, 512]) for i in range(2)])
                pTr = Ring([c.sb(es, "a_pT%d" % i, [128, 512], F32R) for i in range(4)])
                osb = c.sb(es, "a_osb", [65, 512])
                rec = c.sb(es, "a_rec", [64, 512])
                att = Ring([c.sb(es, "a_att%d" % i, [64, 512]) for i in range(2)])
                scale = 96.0 ** -0.5
                c.memset_r(wkt[:], 0.0)
                c.memset_r(V[:], 1.0)
                for hh in range(8):
                    c.dma(wq[:], wuq[hh], q="gpsimd")
                    c.dma(wqs[:], wuqs[hh], q="gpsimd")
                    c.dma(wkt[:, 0:64], wk[hh], q="gpsimd")
                    c.dma(wvt[:], wv[hh], q="gpsimd")
                    for i in range(NT):
                        tl = slice(i * 512, (i + 1) * 512)
                        for kc in range(2):
                            c.mm(P[0][0:96, :], wq[:, kc, :], qn[:, kc, tl], start=(kc == 0), stop=(kc == 1))
                        for kc in range(2):
                            c.mm(P[1][0:96, :], wqs[:, kc, :], qn[:, kc, tl], start=(kc == 0), stop=(kc == 1))
                        c.mm(P[2][0:96, :], wkt[:, :], kvn[:, tl], start=True, stop=False)
                        c.mm(P[2][0:96, :], ipe[:, 0, :], kpe[:, tl], start=False, stop=True)
                        c.mm(P[3][0:96, :], ipe[:, 1, :], kpe[:, tl], start=True, stop=True)
                        qk = ((P[0], P[1], qh, qTt, sq96, rs96, P[4]), (P[2], P[3], kh, kTt, sq96b, rs96b, P[5]))
                        for (pa, pb, gv, dst, sq_, rs_, pn) in qk:
                            c.act(sq_[:, :], pa[0:96, :], AF.Square)
                        for (pa, pb, gv, dst, sq_, rs_, pn) in qk:
                            c.mm(pn[0:96, :], ones_r[0:96, 0:96], sq_[:, :])
                        for (pa, pb, gv, dst, sq_, rs_, pn) in qk:
                            c.ts(rs_[0:96, :], pn[0:96, :], 1.0 / 96.0, EPS, ALU.mult, ALU.add)
                        for (pa, pb, gv, dst, sq_, rs_, pn) in qk:
                            c.act(rs_[0:96, :], rs_[0:96, :], AF.Ln)
                        for (pa, pb, gv, dst, sq_, rs_, pn) in qk:
                            c.act(rs_[0:96, :], rs_[0:96, :], AF.Exp, scale=-0.5)
                        tq = []
                        for (pa, pb, gv, dst, sq_, rs_, pn) in qk:
                            t1 = t1r.next()
                            t2 = t2r.next()
                            tq.append((t1, t2))
                            c.stt(t1[:, :], pa[0:96, :], gv[:, 0:1], cosT[:, tl], ALU.mult, ALU.mult)
                            c.stt(t2[:, :], pb[0:96, :], gv[:, 1:2], sinT[:, tl], ALU.mult, ALU.mult)
                        for (pa, pb, gv, dst, sq_, rs_, pn), (t1, t2) in zip(qk, tq):
                            c.tt(t1[:, :], t1[:, :], t2[:, :], ALU.add)
                        for (pa, pb, gv, dst, sq_, rs_, pn), (t1, t2) in zip(qk, tq):
                            c.tt(dst[:, tl], t1[:, :], rs_[0:96, :], ALU.mult)
                    for j in range(S // 128):
                        c.mm(P[5][:, 0:64], kvn[:, j * 128:(j + 1) * 128], wvt[:, :])
                        c.copy(V[:, j, 0:64], P[5][:, 0:64], eng="scalar")
                    units = [(qi, kj) for qi in range(NT) for kj in range(4 * (qi + 1))]
                    sbanks = [P[0], P[1], P[6], P[7]]

                    def issue_s(u):
                        qi_, kj_ = units[u]
                        c.mm(sbanks[u % 4][:, :], kTt[:, kj_ * 128:(kj_ + 1) * 128], qTt[:, qi_ * 512:(qi_ + 1) * 512])

                    for u in range(min(2, len(units))):
                        issue_s(u)
                    for u, (qi, kj) in enumerate(units):
                        if u + 2 < len(units):
                            issue_s(u + 2)
                        ql = slice(qi * 512, (qi + 1) * 512)
                        po = P[4 + (qi % 2)]
                        nk = 4 * (qi + 1)
                        pT = pTr.next()
                        c.act(pT[:, :], sbanks[u % 4][:, :], AF.Exp, scale=scale)
                        if kj >= 4 * qi:
                            c.tt(pT[:, :], pT[:, :].bc(F32), cmask[:, kj - 4 * qi, :], ALU.mult)
                        c.mm(po[0:65, :], V[:, kj, :], pT[:, :], start=(kj == 0), stop=(kj == nk - 1))
                        if kj == nk - 1:
                            c.copy(osb[:, :], po[0:65, :], eng="scalar")
                            c.mm(P[2][0:64, :], sel65[:, :], osb[:, :])
                            c.recip(rec[:, :], P[2][0:64, :])
                            a_ = att.next()
                            c.tt(a_[:, :], osb[0:64, :], rec[:, :], ALU.mult)
                            c.dma(mixT[hh * 64:(hh + 1) * 64, ql], a_[:])
                c.barrier()

        if "s5" in phases:
            with ExitStack() as es:
                sp = c.sb(es, "s_p", [128, 3, 16])
                c.dma(sp[:], s5p_d[:])
                names = ["dt", "lr", "mag", "ang", "sinA", "cosA", "abr", "abi", "nabi", "den", "nr", "fr", "fi",
                         "nfi", "t0", "t1", "pr", "pi", "npi", "t2"]
                v = {n: c.sb(es, "s_" + n, [128, 16]) for n in names}
                A = lambda n: v[n][:, :]
                li = sp[:, 1, :]
                c.act(A("dt"), sp[:, 2, :], AF.Exp)
                c.ts(A("lr"), sp[:, 0, :], -1e-4, None, ALU.min)
                c.tt(A("t0"), A("lr"), A("dt"), ALU.mult)
                c.act(A("mag"), A("t0"), AF.Exp)
                c.tt(A("ang"), li, A("dt"), ALU.mult)
                with ExitStack() as es2:
                    range_reduce_sin(c, es2, A("ang"), A("sinA"), A("cosA"), [128, 16], "srr")
                    c.barrier()
                c.tt(A("abr"), A("mag"), A("cosA"), ALU.mult)
                c.tt(A("abi"), A("mag"), A("sinA"), ALU.mult)
                c.ts(A("nabi"), A("abi"), -1.0)
                c.tt(A("den"), A("lr"), A("lr"), ALU.mult)
                c.tt(A("t0"), li, li, ALU.mult)
                c.tt(A("den"), A("den"), A("t0"), ALU.add)
                c.recip(A("den"), A("den"))
                c.ts(A("nr"), A("abr"), -1.0, None, ALU.add)
                c.tt(A("t0"), A("nr"), A("lr"), ALU.mult)
                c.tt(A("t1"), A("abi"), li, ALU.mult)
                c.tt(A("t0"), A("t0"), A("t1"), ALU.add)
                c.tt(A("fr"), A("t0"), A("den"), ALU.mult)
                c.tt(A("t0"), A("abi"), A("lr"), ALU.mult)
                c.tt(A("t1"), A("nr"), li, ALU.mult)
                c.tt(A("t0"), A("t0"), A("t1"), ALU.subtract)
                c.tt(A("fi"), A("t0"), A("den"), ALU.mult)
                c.ts(A("nfi"), A("fi"), -1.0)
                c.copy(A("pr"), A("cosA"))
                c.ts(A("pi"), A("sinA"), -1.0)
                c.copy(A("npi"), A("sinA"))

                BT = c.sb(es, "s_BT", [128, 16, 2, 128], F32R)
                CP = c.sb(es, "s_CP", [128, 16, 2, 128], F32R)
                Er = c.sb(es, "s_Er", [128, 16, 512])
                Ei = c.sb(es, "s_Ei", [128, 16, 512])
                with ExitStack() as es2:
                    sb_ = c.sb(es2, "s_b", [128, 2, 16, 16])
                    sc_ = c.sb(es2, "s_c", [128, 2, 16, 16])
                    c.dma(sb_[:], s5b_d[:])
                    c.dma(sc_[:], s5c_d[:])
                    bb = c.sb(es2, "s_bb", [128, 2, 16, 16])
                    tmp = c.sb(es2, "s_tmp", [128, 16])
                    BP = c.sb(es2, "s_BP", [128, 16, 2, 128])
                    c.memset(BP[:], 0.0)
                    c.memset_r(CP[:], 0.0)
                    for cc in range(16):
                        c.ts(tmp[:, :], sb_[:, 0, cc, :], v["fr"][:, cc:cc + 1])
                        c.stt(bb[:, 0, cc, :], sb_[:, 1, cc, :], v["nfi"][:, cc:cc + 1], tmp[:, :], ALU.mult, ALU.add)
                        c.ts(tmp[:, :], sb_[:, 1, cc, :], v["fr"][:, cc:cc + 1])
                        c.stt(bb[:, 1, cc, :], sb_[:, 0, cc, :], v["fi"][:, cc:cc + 1], tmp[:, :], ALU.mult, ALU.add)
                        offs = (((2 * cc) % 8) * 16, ((2 * cc + 1) % 8) * 16)
                        for half in range(2):
                            ps_ = slice(half * 64, (half + 1) * 64)
                            o = offs[half]
                            for ri in range(2):
                                c.copy(BP[ps_, cc, ri, o:o + 16], bb[ps_, ri, cc, :])
                            c.copy(CP[ps_, cc, 0, o:o + 16], sc_[ps_, 0, cc, :])
                            c.ts(CP[ps_, cc, 1, o:o + 16], sc_[ps_, 1, cc, :], -1.0)
                        for ri in range(2):
                            pb = P[(cc * 2 + ri) % 4]
                            c.transpose(pb[:, 0:128], BP[:, cc, ri, :], ident[:, :])
                            c.copy(BT[:, cc, ri, :], pb[:, 0:128], eng="scalar")
                    c.barrier()
                c.memset(Er[:, :, 0:1], 1.0)
                c.memset(Ei[:, :, 0:1], 0.0)
                tA = c.sb(es, "s_tA", [128, 256])
                tB = c.sb(es, "s_tB", [128, 256])
                m = 1
                while m < 512:
                    for cc in range(16):
                        c.ts(tA[:, 0:m], Er[:, cc, 0:m], v["pr"][:, cc:cc + 1])
                        c.stt(Er[:, cc, m:2 * m], Ei[:, cc, 0:m], v["npi"][:, cc:cc + 1], tA[:, 0:m], ALU.mult, ALU.add)
                        c.ts(tB[:, 0:m], Er[:, cc, 0:m], v["pi"][:, cc:cc + 1])
                        c.stt(Ei[:, cc, m:2 * m], Ei[:, cc, 0:m], v["pr"][:, cc:cc + 1], tB[:, 0:m], ALU.mult, ALU.add)
                    m *= 2
                    if m < 512:
                        c.tt(A("t0"), A("pr"), A("pr"), ALU.mult)
                        c.tt(A("t1"), A("pi"), A("pi"), ALU.mult)
                        c.tt(A("t2"), A("pr"), A("pi"), ALU.mult)
                        c.tt(A("pr"), A("t0"), A("t1"), ALU.subtract)
                        c.ts(A("pi"), A("t2"), 2.0)
                        c.ts(A("npi"), A("t2"), -2.0)
                dsk = c.sb(es, "s_dsk", [128, 4])
                glub = c.sb(es, "s_glub", [128, 4])
                gluw = c.sb(es, "s_gluw", [128, 4, 512], F32R)
                c.dma(dsk[:], s5d_d[:])
                c.dma(glub[:], glub_d[:])
                c.dma(gluw[:], gluw_d[:, :].re("(kc p) f -> p kc f", p=128), q="gpsimd")
                ones512 = c.sb(es, "s_ones", [128, 512])
                c.memset(ones512[:], 1.0)
                xprev = c.sb(es, "s_xprev", [128, 16, 2])
                utr = Ring([c.sb(es, "s_ut%d" % i, [128, 4, 512], F32R) for i in range(2)])
                tr_ = [Ring([c.sb(es, "s_w%d_%d" % (k, i), [128, 512]) for i in range(2)]) for k in range(7)]
                tr2_ = [Ring([c.sb(es, "s_v%d_%d" % (k, i), [128, 512]) for i in range(2)]) for k in range(2)]
                xrr = Ring([c.sb(es, "s_xr%d" % i, [128, 512], F32R) for i in range(2)])
                xir = Ring([c.sb(es, "s_xi%d" % i, [128, 512], F32R) for i in range(2)])
                cr_ = c.sb(es, "s_cr", [128, 2])
                yy = c.sb(es, "s_yy", [128, 512])
                g1 = c.sb(es, "s_g1", [128, 512])
                g2 = c.sb(es, "s_g2", [128, 512])
                gt = c.sb(es, "s_gt", [128, 4, 512], F32R)
                gf = c.sb(es, "s_gf", [128, 512])
                sg = c.sb(es, "s_sg", [128, 512])
                ot = c.sb(es, "s_ot", [128, 4, 512])
                for n in range(NT):
                    ut = utr.next()
                    c.dma(ut[:], fm(uT, n, 4), q="gpsimd")
                    for cc in range(16):
                        uc = cc // 4
                        pdr, pdi = P[0 + 2 * (cc % 2)], P[1 + 2 * (cc % 2)]
                        c.mm(pdr[:, :], BT[:, cc, 0, :], ut[:, uc, :])
                        c.mm(pdi[:, :], BT[:, cc, 1, :], ut[:, uc, :])
                        a1, a2, b1, b2, rt, sr, si = [r.next() for r in tr_]
                        c.tt(a1[:, :], pdr[:, :], Er[:, cc, :], ALU.mult)
                        c.tt(a2[:, :], pdi[:, :], Ei[:, cc, :], ALU.mult)
                        c.tt(a1[:, :], a1[:, :], a2[:, :], ALU.subtract)
                        c.tt(b1[:, :], pdr[:, :], Ei[:, cc, :], ALU.mult)
                        c.tt(b2[:, :], pdi[:, :], Er[:, cc, :], ALU.mult)
                        c.tt(b1[:, :], b1[:, :], b2[:, :], ALU.add)
                        if n > 0:
                            c.ts(cr_[:, 0:1], xprev[:, cc, 0:1], v["abr"][:, cc:cc + 1])
                            c.stt(cr_[:, 0:1], xprev[:, cc, 1:2], v["nabi"][:, cc:cc + 1], cr_[:, 0:1], ALU.mult, ALU.add)
                            c.ts(cr_[:, 1:2], xprev[:, cc, 0:1], v["abi"][:, cc:cc + 1])
                            c.stt(cr_[:, 1:2], xprev[:, cc, 1:2], v["abr"][:, cc:cc + 1], cr_[:, 1:2], ALU.mult, ALU.add)
                            c.tt(a1[:, 0:1], a1[:, 0:1], cr_[:, 0:1], ALU.add)
                            c.tt(b1[:, 0:1], b1[:, 0:1], cr_[:, 1:2], ALU.add)
                        c.ts(rt[:, :], ones512[:, :], v["mag"][:, cc:cc + 1])
                        c.scan(sr[:, :], rt[:, :], a1[:, :])
                        c.scan(si[:, :], rt[:, :], b1[:, :])
                        xr, xi = xrr.next(), xir.next()
                        a3, b3 = tr2_[0].next(), tr2_[1].next()
                        c.tt(a2[:, :], sr[:, :], Er[:, cc, :], ALU.mult, eng="gpsimd")
                        c.tt(b2[:, :], si[:, :], Ei[:, cc, :], ALU.mult, eng="gpsimd")
                        c.tt(a3[:, :], si[:, :], Er[:, cc, :], ALU.mult, eng="gpsimd")
                        c.tt(b3[:, :], sr[:, :], Ei[:, cc, :], ALU.mult, eng="gpsimd")
                        c.tt(xr[:, :], a2[:, :], b2[:, :], ALU.add)
                        c.tt(xi[:, :], a3[:, :], b3[:, :], ALU.subtract)
                        c.copy(xprev[:, cc, 0:1], xr[:, 511:512].bc(F32))
                        c.copy(xprev[:, cc, 1:2], xi[:, 511:512].bc(F32))
                        py = P[4 + (uc % 2)]
                        c.mm(py[:, :], CP[:, cc, 0, :], xr[:, :], start=(cc % 4 == 0), stop=False)
                        c.mm(py[:, :], CP[:, cc, 1, :], xi[:, :], start=False, stop=(cc % 4 == 3))
                        if cc % 4 == 3:
                            c.stt(yy[:, :], ut[:, uc, :].bc(F32), dsk[:, uc:uc + 1], py[:, :], ALU.mult, ALU.add)
                            gelu_tanh(c, gf[:, :], yy[:, :], g1[:, :], g2[:, :])
                            c.copy(gt[:, uc, :], gf[:, :], eng="scalar")
                    for fo in range(4):
                        pg = P[6 + (fo % 2)]
                        for kc in range(4):
                            c.mm(pg[:, :], gluw[:, kc, fo * 128:(fo + 1) * 128], gt[:, kc, :], start=(kc == 0), stop=(kc == 3))
                        c.act(sg[:, :], pg[:, :], AF.Sigmoid, bias=glub[:, fo:fo + 1])
                        c.tt(ot[:, fo, :], gt[:, fo, :].bc(F32), sg[:, :], ALU.mult)
                    c.dma(mixT[512:1024, n * 512:(n + 1) * 512].re("(kc p) t -> p kc t", p=128), ot[:])
                c.barrier()

        if "e4" in phases:
            with ExitStack() as es:
                wo = c.sb(es, "e4w", [128, 8, 1024], F32R)
                c.dma(wo[:], e_wout[:, :].re("(kc p) f -> p kc f", p=128), q="gpsimd")
                mr = Ring([c.sb(es, "e4m%d" % i, [128, 8, 512], F32R) for i in range(2)])
                xr_ = Ring([c.sb(es, "e4x%d" % i, [128, 8, 512]) for i in range(2)])
                orr = Ring([c.sb(es, "e4o%d" % i, [128, 8, 512]) for i in range(2)])
                for i in range(NT):
                    mt = mr.next()
                    xt = xr_.next()
                    c.dma(mt[:], fm(mixT, i, 8), q="gpsimd")
                    c.dma(xt[:], fm(xT, i, 8))
                    o = orr.next()
                    for dc in range(8):
                        pb = P[dc % 4]
                        for kc in range(8):
                            c.mm(pb[:, :], wo[:, kc, dc * 128:(dc + 1) * 128], mt[:, kc, :], start=(kc == 0), stop=(kc == 7))
                        c.stt(o[:, dc, :], pb[:, :], mod[:, 16 + dc:17 + dc], xt[:, dc, :], ALU.mult, ALU.add)
                    c.dma(fm(x1T, i, 8), o[:])
                c.barrier()

        def moe_phase(l, xin, xout):
            with ExitStack() as es:
                st = mk_norm(es, "mn")
                xr_ = Ring([c.sb(es, "mx%d" % i, [128, 8, 512]) for i in range(1)])
                hb = c.sb(es, "mh", [128, 8, 512], F32R)
                hf = c.sb(es, "mhf", [128, 8, 512])
                hr = c.sb(es, "mhr", [128, 8, 512], F32R)
                rw = c.sb(es, "mrw", [128, 8, NE])
                rb = c.sb(es, "mrb", [1, NE])
                bg = c.sb(es, "mbg", [128, NE, 16])
                bg2 = c.sb(es, "mbg2", [128, NE, 8])
                bd = c.sb(es, "mbd", [NE, D])
                sel = c.sb(es, "msel", [NE, NE, 128])
                c.dma(rw[:], router_w[l].re("(kc p) e -> p kc e", p=128))
                c.dma(rb[:], router_b[l:l + 1, :])
                c.dma(bg[:], bgu[l])
                c.dma(bd[:], bdn[l])
                c.dma(sel[:], sel_d[:])
                c.ts(bg2[:, :, :], bg[:, :, 0:8], 1.702)
                lg = c.sb(es, "mlg", [128, NE])
                mx8 = c.sb(es, "mmx8", [128, 8])
                nmx = c.sb(es, "mnmx", [128, 1])
                msk = c.sb(es, "mmsk", [128, NE])
                ex = c.sb(es, "mex", [128, NE])
                ssum = c.sb(es, "mss", [128, 1])
                gts = c.sb(es, "mgts", [128, NE])
                gT = c.sb(es, "mgT", [NE, 512])
                gbc = Ring([c.sb(es, "mgbc%d" % i, [128, 512]) for i in range(2)])
                wgr = Ring([c.sb(es, "mwg%d" % i, [128, 8, 256], F32R) for i in range(3)])
                wdr = Ring([c.sb(es, "mwd%d" % i, [128, 1024], F32R) for i in range(12)])
                silr = Ring([c.sb(es, "msil%d" % i, [128, 512]) for i in range(2)])
                l1r = Ring([c.sb(es, "ml1%d" % i, [128, 512]) for i in range(2)])
                c7 = 7.0 / (1.0 + np.exp(-1.702 * 7.0)) * 1.702
                for i in range(NT):
                    xt = xr_.next()
                    yacc = xt
                    c.dma(xt[:], fm(xin, i, 8))
                    norm_tile(st, xt, hf, gg[:, l * 2 + 1, :], mod[:, l * 48 + 24:l * 48 + 32], P[7])
                    for kc in range(8):
                        c.copy(hr[:, kc, :], hf[:, kc, :], eng="scalar")
                    for s in range(4):
                        for kc in range(8):
                            c.mm(P[6][:, 0:NE], hf[:, kc, s * 128:(s + 1) * 128], rw[:, kc, :], start=(kc == 0), stop=False)
                        c.mm(P[6][:, 0:NE], ones_f[0:1, :], rb[0:1, :], start=False, stop=True)
                        c.copy(lg[:, :], P[6][:, 0:NE])
                        c.max8(mx8[:, :], lg[:, :])
                        c.ts(nmx[:, :], mx8[:, 0:1], -1.0)
                        c.ts(msk[:, :], lg[:, :], mx8[:, 3:4], None, ALU.is_ge)
                        c.act(ex[:, :], lg[:, :], AF.Exp, bias=nmx[:, 0:1])
                        c.tt(ex[:, :], ex[:, :], msk[:, :], ALU.mult)
                        c.rsum(ssum[:, :], ex[:, :])
                        c.recip(ssum[:, :], ssum[:, :])
                        c.ts(gts[:, :], ex[:, :], ssum[:, 0:1])
                        c.transpose(P[6][0:NE, 128:256], gts[:, :], ident[:, :])
                        c.copy(gT[:, s * 128:(s + 1) * 128], P[6][0:NE, 128:256])
                    for dc in range(8):
                        pb = P[4 + (dc % 2)]
                        c.mm(pb[:, :], bd[:, dc * 128:(dc + 1) * 128], gT[:, :])
                        c.copy(yacc[:, dc, :], pb[:, :], eng=("scalar" if dc % 2 else "vector"))
                    for e in range(NE):
                        gb = gbc.next()
                        c.mm(P[6][:, :], sel[:, e, :], gT[:, :])
                        c.act(gb[:, :], P[6][:, :], AF.Identity, scale=1.0 / 1.702)
                        wds = []
                        for fc in range(8):
                            wg = wgr.next()
                            c.dma(wg[:], wgu[l, e, fc], q="gpsimd")
                            wd = wdr.next()
                            c.dma(wd[:], wdn[l, e, fc * 128:(fc + 1) * 128, :], q="gpsimd", max_dma_last_dim=4096)
                            wds.append(wd)
                            pg, pl = P[0 + 2 * (fc % 2)], P[1 + 2 * (fc % 2)]
                            for kc in range(8):
                                c.mm(pg[:, :], wg[:, kc, 0:128], hr[:, kc, :], start=(kc == 0), stop=(kc == 7))
                            for kc in range(8):
                                c.mm(pl[:, :], wg[:, kc, 128:256], hr[:, kc, :], start=(kc == 0), stop=(kc == 7))
                            sil = silr.next()
                            l1 = l1r.next()
                            c.act(sil[:, :], pg[:, :], AF.Silu, bias=bg2[:, e, fc:fc + 1], scale=1.702)
                            c.ts(l1[:, :], pl[:, :], bg[:, e, 8 + fc:9 + fc], 7.0, ALU.add, ALU.min)
                            c.ts(l1[:, :], l1[:, :], -7.0, 1.0, ALU.max, ALU.add)
                            c.stt(sil[:, :], sil[:, :], c7, l1[:, :], ALU.min, ALU.mult)
                            c.tt(hb[:, fc, :], sil[:, :], gb[:, :], ALU.mult)
                        for dc in range(8):
                            pb = P[4 + (dc % 2)]
                            for fc in range(8):
                                c.mm(pb[:, :], wds[fc][:, dc * 128:(dc + 1) * 128], hb[:, fc, :], start=(fc == 0), stop=(fc == 7))
                            c.tt(yacc[:, dc, :], yacc[:, dc, :], pb[:, :], ALU.add)
                    c.dma(hf[:], fm(xin, i, 8))
                    for dc in range(8):
                        c.stt(hf[:, dc, :], yacc[:, dc, :], mod[:, l * 48 + 40 + dc:l * 48 + 41 + dc], hf[:, dc, :],
                              ALU.mult, ALU.add)
                    c.dma(fm(xout, i, 8), hf[:])
                c.barrier()


        def moe_sparse(l, xin, xout):
            wgu_rows = wgu[:, :]
            wdn_rows = wdn[:, :]
            bgu_rows = View(bgu2, bgu2.t.rearrange("l r c -> (l r) c"))
            bdn_rows = View(bdn2, bdn2.t.rearrange("l r c -> (l r) c"))
            with ExitStack() as esO:
                wixu = c.sb(esO, "q_wixu", [128, NBLK, 4], U32)
                wdxu = c.sb(esO, "q_wdxu", [128, NBLK, 2], U32)
                bixu = c.sb(esO, "q_bixu", [128, NBLK], U32)
                dallu = c.sb(esO, "q_dallu", [128, NSUB, 4], U32)
                beu = c.sb(esO, "q_beu", [128, NBLK], U32)
                with ExitStack() as es:
                    st = mk_norm(es, "qn")
                    xt = c.sb(es, "q_x", [128, 8, 512])
                    hf = c.sb(es, "q_hf", [128, 8, 512])
                    rw = c.sb(es, "q_rw", [128, 8, NE])
                    rb = c.sb(es, "q_rb", [1, NE])
                    us = c.sb(es, "q_us", [128, 128])
                    eio = c.sb(es, "q_eio", [128, NE])
                    jv = c.sb(es, "q_jv", [128, NBLK])
                    tk = c.sb(es, "q_tk", [128, NSUB])
                    wid = c.sb(es, "q_wid", [128, 9])
                    c.dma(rw[:], router_w[l].re("(kc p) e -> p kc e", p=128))
                    c.dma(rb[:], router_b[l:l + 1, :])
                    c.dma(us[:], ustrict_d[:])
                    c.dma(eio[:], eiota_d[:])
                    c.dma(jv[:], jvec_d[:])
                    c.dma(tk[:], tokid_d[:])
                    c.dma(wid[:], widx_d[:])
                    ri0 = c.sb(es, "q_ri0", [128, NBLK * 4, 2])
                    c.dma(ri0[:], rowinit_d[:])
                    c.dma(rowinfo[:, :].re("(p a) c -> p a c", p=128), ri0[:])
                    zt = c.sb(es, "q_zt", [128, D])
                    c.memset(zt[:], 0.0)
                    for a in range(4):
                        c.dma(hrows[S + a * 128:S + (a + 1) * 128, :], zt[:])
                    macc = c.sb(es, "q_macc", [128, NSUB + 1, NE])
                    c.memset(macc[:, 0, :], 0.0)
                    tmpr = [Ring([c.sb(es, "q_tmp%d_%d" % (k, i), shp, dt_) for i in range(2)])
                            for k, (shp, dt_) in enumerate([([128, 4, NE], F32), ([128, 4, 8], F32), ([128, 4, 8], U32),
                                                            ([128, 4], F32), ([128, 4, NE], F32), ([128, 4, 4], F32),
                                                            ([128, 4], F32)])]
                    cumall = c.sb(es, "q_cum", [128, NSUB, NE])
                    idxf = c.sb(es, "q_idxf", [128, NSUB, 4])
                    gk = c.sb(es, "q_gk", [128, NSUB, 4])
                    hrr = Ring([c.sb(es, "q_hr%d" % i, [128, D]) for i in range(2)])
                    lg = c.sb(es, "q_lg", [128, NE])
                    mx8 = c.sb(es, "q_mx8", [128, 8])
                    ix8 = c.sb(es, "q_ix8", [128, 8], U32)
                    nmx = c.sb(es, "q_nmx", [128, 1])
                    msk = c.sb(es, "q_msk", [128, NE])
                    ex4 = c.sb(es, "q_ex4", [128, 4])
                    s4 = c.sb(es, "q_s4", [128, 1])
                    for i in range(NT):
                        c.dma(xt[:], fm(xin, i, 8))
                        norm_tile(st, xt, hf, gg[:, l * 2 + 1, :], mod[:, l * 48 + 24:l * 48 + 32], P[7])
                        lga, mxa, ixa, nma, mka, exa, s4a = [r.next() for r in tmpr]
                        for s_ in range(4):
                            g = 4 * i + s_
                            tsl = slice(s_ * 128, (s_ + 1) * 128)
                            for kc in range(8):
                                c.transpose(P[kc // 4][:, (kc % 4) * 128:(kc % 4 + 1) * 128], hf[:, kc, tsl], ident[:, :])
                            hr_ = hrr.next()
                            c.copy(hr_[:, 0:512], P[0][:, :], eng="scalar")
                            c.copy(hr_[:, 512:1024], P[1][:, :])
                            c.dma(hrows[g * 128:(g + 1) * 128, :], hr_[:])
                            for kc in range(8):
                                c.mm(P[6][:, s_ * NE:(s_ + 1) * NE], hf[:, kc, tsl], rw[:, kc, :], start=(kc == 0), stop=False)
                            c.mm(P[6][:, s_ * NE:(s_ + 1) * NE], ones_f[0:1, :], rb[0:1, :], start=False, stop=True)
                        c.copy(lga[:, :, :], P[6][:, 0:4 * NE].re("p (q e) -> p q e", q=4))
                        for s_ in range(4):
                            c.max8(mxa[:, s_, :], lga[:, s_, :])
                        for s_ in range(4):
                            c.max_index(ixa[:, s_, :], mxa[:, s_, :], lga[:, s_, :])
                        c.copy(idxf[:, 4 * i:4 * i + 4, :], ixa[:, :, 0:4])
                        c.ts(nma[:, :], mxa[:, :, 0], -1.0)
                        for s_ in range(4):
                            c.ts(mka[:, s_, :], lga[:, s_, :], mxa[:, s_, 3:4], None, ALU.is_ge)
                        for s_ in range(4):
                            c.act(exa[:, s_, :], mxa[:, s_, 0:4], AF.Exp, bias=nma[:, s_:s_ + 1])
                        for s_ in range(4):
                            g = 4 * i + s_
                            c.mm(P[5][:, s_ * NE:(s_ + 1) * NE], us[:, :], mka[:, s_, :], start=True, stop=False)
                            c.mm(P[5][:, s_ * NE:(s_ + 1) * NE], ones_f[:, :], macc[:, g, :], start=False, stop=True)
                            c.tt(macc[:, g + 1, :], macc[:, g, :], mka[:, s_, :], ALU.add)
                        for s_ in range(4):
                            c.rsum(s4a[:, s_:s_ + 1], exa[:, s_, :])
                        c.recip(s4a[:, :], s4a[:, :])
                        for s_ in range(4):
                            c.ts(gk[:, 4 * i + s_, :], exa[:, s_, :], s4a[:, s_:s_ + 1])
                        c.copy(cumall[:, 4 * i:4 * i + 4, :], P[5][:, 0:4 * NE].re("p (q e) -> p q e", q=4), eng="scalar")
                    cnt = c.sb(es, "q_cnt", [128, NE])
                    qi_ = c.sb(es, "q_qi", [128, NE], I32)
                    pad = c.sb(es, "q_pad", [128, NE])
                    incl = c.sb(es, "q_incl", [128, NE])
                    pstart = c.sb(es, "q_pstart", [128, NE])
                    one32 = c.sb(es, "q_one32", [128, NE])
                    c.memset(one32[:], 1.0)
                    c.mm(P[5][:, 0:NE], ones_f[:, :], macc[:, NSUB, :])
                    c.ts(cnt[:, :], P[5][:, 0:NE], 511.0, 1.0 / 512, ALU.add, ALU.mult)
                    c.ts(cnt[:, :], cnt[:, :], -0.5 + 1.0 / 1024, None, ALU.add)
                    c.copy(qi_[:, :], cnt[:, :])
                    c.copy(pad[:, :], qi_[:, :])
                    c.scan(incl[:, :], one32[:, :], pad[:, :])
                    c.tt(pstart[:, :], incl[:, :], pad[:, :], ALU.subtract)
                    c.ts(pstart[:, :], pstart[:, :], 512.0)
                    bef = c.sb(es, "q_bef", [128, NBLK])
                    c.memset(bef[:], 0.0)
                    for e in range(NE):
                        c.stt(bef[:, :], jv[:, :], incl[:, e:e + 1], bef[:, :], ALU.is_ge, ALU.add)
                    c.ts(bef[:, :], bef[:, :], float(NE - 1), None, ALU.min)
                    if l > 0:
                        befl = c.sb(es, "q_befl", [128, NBLK])
                        c.ts(befl[:, :], bef[:, :], float(l * NE), None, ALU.add)
                        c.copy(beu[:, :], befl[:, :])
                    else:
                        c.copy(beu[:, :], bef[:, :])
                    wixf = c.sb(es, "q_wixf", [128, NBLK, 4])
                    for g_ in range(4):
                        c.ts(wixf[:, :, g_], bef[:, :], 512.0, wid[:, g_:g_ + 1], ALU.mult, ALU.add)
                    if l > 0:
                        c.ts(wixf[:, :, :], wixf[:, :, :], float(l * NE * 512), None, ALU.add)
                    c.copy(wixu[:, :, :], wixf[:, :, :])
                    for h_ in range(2):
                        c.ts(wixf[:, :, h_], bef[:, :], 256.0, wid[:, h_:h_ + 1], ALU.mult, ALU.add)
                    if l > 0:
                        c.ts(wixf[:, :, 0:2], wixf[:, :, 0:2], float(l * NE * 256), None, ALU.add)
                    c.copy(wdxu[:, :, :], wixf[:, :, 0:2])
                    c.ts(bef[:, :], bef[:, :], 128.0, wid[:, 8:9], ALU.mult, ALU.add)
                    if l > 0:
                        c.ts(bef[:, :], bef[:, :], float(l * NE * 128), None, ALU.add)
                    c.copy(bixu[:, :], bef[:, :])
                    dall = c.sb(es, "q_dall", [128, NSUB, 4])
                    df = c.sb(es, "q_df", [128, NE])
                    oh = c.sb(es, "q_oh", [128, NE])
                    for g in range(NSUB):
                        c.tt(df[:, :], pstart[:, :], cumall[:, g, :], ALU.add)
                        for k in range(4):
                            c.ts(oh[:, :], eio[:, :], idxf[:, g, k:k + 1], None, ALU.is_equal)
                            c.tt(oh[:, :], oh[:, :], df[:, :], ALU.mult)
                            c.rsum(dall[:, g, k:k + 1], oh[:, :])
                    c.copy(dallu[:, :, :], dall[:, :, :])
                    sa = c.sb(es, "q_sa", [128, NSUB, 4])
                    sai = c.sb(es, "q_sai", [128, NSUB, 4], I32)
                    sp_ = c.sb(es, "q_sp", [128, NSUB, 4])
                    sidxu = c.sb(es, "q_sidxu", [128, NSUB, 4], U32)
                    c.ts(sa[:, :, :], dall[:, :, :], 1.0 / 128, -0.5 + 1.0 / 256, ALU.mult, ALU.add)
                    c.copy(sai[:, :, :], sa[:, :, :])
                    c.copy(sa[:, :, :], sai[:, :, :])
                    c.stt(sp_[:, :, :], sa[:, :, :], -128.0, dall[:, :, :], ALU.mult, ALU.add)
                    c.stt(sp_[:, :, :], sp_[:, :, :], float(NBLK * 4), sa[:, :, :], ALU.mult, ALU.add)
                    c.copy(sidxu[:, :, :], sp_[:, :, :])
                    pay = c.sb(es, "q_pay", [128, NSUB, 4, 2])
                    for k in range(4):
                        c.copy(pay[:, :, k, 0], tk[:, :])
                    c.copy(pay[:, :, :, 1], gk[:, :, :])
                    PQ = c.engs["gpsimd"]
                    for g in range(NSUB):
                        for k in range(4):
                            c.scatter(rowinfo[:, :], pay[:, g, k, :], sidxu[:, g, k:k + 1])
                        if g % 8 == 7:
                            d_ = pay.dsem["sw"]
                            PQ["h"].wait_ge(d_["sem"], d_["cnt"])
                            PQ["waited"][d_["key"]] = d_["cnt"]
                    c.barrier()
                with ExitStack() as es:
                    rir = Ring([c.sb(es, "b_ri%d" % i, [128, 4, 2]) for i in range(3)])
                    riur = Ring([c.sb(es, "b_riu%d" % i, [128, 4], U32) for i in range(3)])
                    xgr = Ring([c.sb(es, "b_xg%d" % i, [128, 4, D]) for i in range(3)])
                    xbT = c.sb(es, "b_xbT", [128, 8, 512], BF16)
                    at = c.sb(es, "b_a", [128, 8, 512], BF16)
                    yrow = c.sb(es, "b_yrow", [128, 4, D])
                    bbr = Ring([c.sb(es, "b_bb%d" % i, [128, D]) for i in range(3)])
                    bgr = Ring([c.sb(es, "b_bg%d" % i, [128, 16]) for i in range(3)])
                    bg2r = Ring([c.sb(es, "b_bg2%d" % i, [128, 8]) for i in range(3)])
                    bdr = Ring([c.sb(es, "b_bd%d" % i, [128, 8]) for i in range(3)])
                    stgr = Ring([c.sb(es, "b_stg%d" % i, [128, 4096]) for i in range(3)])
                    wgr = Ring([c.sb(es, "b_wg%d" % i, [128, 8, 256], BF16) for i in range(6)])
                    wdr = Ring([c.sb(es, "b_wd%d" % i, [128, 1024], BF16) for i in range(10)])
                    silr = Ring([c.sb(es, "b_sil%d" % i, [128, 512]) for i in range(2)])
                    l1r = Ring([c.sb(es, "b_l1%d" % i, [128, 512]) for i in range(2)])
                    c7 = 7.0 / (1.0 + np.exp(-1.702 * 7.0)) * 1.702
                    riall = c.sb(es, "b_riall", [128, NBLK * 4, 2])
                    riuall = c.sb(es, "b_riuall", [128, NBLK * 4], U32)
                    c.dma(riall[:], rowinfo[:, :].re("(p a) c -> p a c", p=128))
                    c.copy(riuall[:, :], riall[:, :, 0])

                    def prefetch(jn):
                        xg_ = xgr.next()
                        for a in range(4):
                            c.gather(xg_[:, a, :], hrows[:, :], riuall[:, jn * 4 + a:jn * 4 + a + 1])
                        bgt_, bg2_ = bgr.next(), bg2r.next()
                        c.gather(bgt_[:, :], bgu_rows, bixu[:, jn:jn + 1])
                        c.ts(bg2_[:, :], bgt_[:, 0:8], 1.702)
                        bb_ = bbr.next()
                        c.gather(bb_[:, :], bdnr_d[:, :], beu[:, jn:jn + 1])
                        return xg_, jn, bgt_, bg2_, bb_

                    def job_g(jn, g_, wgs):
                        stg = stgr.next()
                        c.gather(stg[:, :], wgu_rows, wixu[:, jn, g_:g_ + 1])
                        for fl in range(2):
                            wg = wgr.next()
                            c.copy(wg[:, :, :].re("p kc j -> p (kc j)"), stg[:, fl * 2048:(fl + 1) * 2048], eng="scalar")
                            wgs[2 * g_ + fl] = wg

                    def job_d(jn, h_, wds):
                        stg = stgr.next()
                        c.gather(stg[:, :], wdn_rows, wdxu[:, jn, h_:h_ + 1])
                        for fl in range(4):
                            wd = wdr.next()
                            c.copy(wd[:, :], stg[:, fl * 1024:(fl + 1) * 1024])
                            wds[4 * h_ + fl] = wd

                    def trans_in(xg_):
                        for kc in range(8):
                            pb = P[6 + (kc % 2)]
                            for a in range(4):
                                c.transpose(pb[:, a * 128:(a + 1) * 128], xg_[:, a, kc * 128:(kc + 1) * 128], ident[:, :])
                            c.copy(xbT[:, kc, :], pb[:, :], eng=("scalar" if kc % 2 else "vector"))

                    def stage_b(fc, wg, bgt, bg2):
                        pg, pl = P[0 + 2 * (fc % 2)], P[1 + 2 * (fc % 2)]
                        for kc in range(8):
                            c.mm(pg[:, :], wg[:, kc, 0:128], xbT[:, kc, :], start=(kc == 0), stop=(kc == 7))
                        for kc in range(8):
                            c.mm(pl[:, :], wg[:, kc, 128:256], xbT[:, kc, :], start=(kc == 0), stop=(kc == 7))
                        sil = silr.next()
                        l1 = l1r.next()
                        c.act(sil[:, :], pg[:, :], AF.Silu, bias=bg2[:, fc:fc + 1], scale=1.702)
                        c.ts(l1[:, :], pl[:, :], bgt[:, 8 + fc:9 + fc], 7.0, ALU.add, ALU.min)
                        c.ts(l1[:, :], l1[:, :], -7.0, 1.0, ALU.max, ALU.add)
                        c.stt(sil[:, :], sil[:, :], c7, l1[:, :], ALU.min, ALU.mult)
                        c.ts(at[:, fc, :], sil[:, :], 1.0 / 1.702)

                    cur = prefetch(0)
                    trans_in(cur[0])
                    wgs, wds = [None] * 8, [None] * 8
                    job_g(0, 0, wgs)
                    job_g(0, 1, wgs)
                    for j in range(NBLK):
                        xg, ri, bgt, bg2, bdt = cur
                        if j + 1 < NBLK:
                            nxt = prefetch(j + 1)
                        stage_b(0, wgs[0], bgt, bg2)
                        stage_b(1, wgs[1], bgt, bg2)
                        job_d(j, 0, wds)
                        job_g(j, 2, wgs)
                        stage_b(2, wgs[2], bgt, bg2)
                        stage_b(3, wgs[3], bgt, bg2)
                        job_g(j, 3, wgs)
                        job_d(j, 1, wds)
                        stage_b(4, wgs[4], bgt, bg2)
                        stage_b(5, wgs[5], bgt, bg2)
                        stage_b(6, wgs[6], bgt, bg2)
                        stage_b(7, wgs[7], bgt, bg2)
                        wds_j = list(wds)
                        if j + 1 < NBLK:
                            trans_in(nxt[0])
                            wgs = [None] * 8
                            job_g(j + 1, 0, wgs)
                            job_g(j + 1, 1, wgs)
                        for a in range(4):
                            for hf_ in range(2):
                                pb = P[4 + ((2 * a + hf_) % 2)]
                                c.mm(pb[:, :], ones_f[0:1, :], bdt[0:1, hf_ * 512:(hf_ + 1) * 512], start=True, stop=False)
                                for fc in range(8):
                                    c.mm(pb[:, :], at[:, fc, a * 128:(a + 1) * 128], wds_j[fc][:, hf_ * 512:(hf_ + 1) * 512],
                                         start=False, stop=(fc == 7))
                                if hf_ == 0:
                                    c.ts(yrow[:, a, 0:512], pb[:, :], riall[:, ri * 4 + a, 1:2])
                                else:
                                    c.act(yrow[:, a, 512:1024], pb[:, :], AF.Identity, scale=riall[:, ri * 4 + a, 1:2])
                        c.dma(yrows[j * 512:(j + 1) * 512, :].re("(a p) d -> p a d", p=128), yrow[:])
                        if j + 1 < NBLK:
                            cur = nxt
                    c.barrier()
                with ExitStack() as es:
                    xt = c.sb(es, "c_x", [128, 8, 512])
                    ykr = Ring([c.sb(es, "c_yk%d" % i, [128, 4, D]) for i in range(4)])
                    tsumr = Ring([c.sb(es, "c_ts%d" % i, [128, D]) for i in range(2)])
                    for i in range(NT):
                        c.dma(xt[:], fm(xin, i, 8))
                        for s_ in range(4):
                            g = 4 * i + s_
                            yk = ykr.next()
                            tsum = tsumr.next()
                            for k in range(4):
                                c.gather(yk[:, k, :], yrows[:, :], dallu[:, g, k:k + 1])
                            c.tt(tsum[:, :], yk[:, 0, :], yk[:, 1, :], ALU.add)
                            c.tt(tsum[:, :], tsum[:, :], yk[:, 2, :], ALU.add)
                            c.tt(tsum[:, :], tsum[:, :], yk[:, 3, :], ALU.add)
                            for kc in range(8):
                                c.transpose(P[kc // 4][:, (kc % 4) * 128:(kc % 4 + 1) * 128],
                                            tsum[:, kc * 128:(kc + 1) * 128], ident[:, :])
                            for kc in range(8):
                                c.stt(xt[:, kc, s_ * 128:(s_ + 1) * 128], P[kc // 4][:, (kc % 4) * 128:(kc % 4 + 1) * 128],
                                      mod[:, l * 48 + 40 + kc:l * 48 + 41 + kc], xt[:, kc, s_ * 128:(s_ + 1) * 128],
                                      ALU.mult, ALU.add)
                        c.dma(fm(xout, i, 8), xt[:])
                    c.barrier()

        moe_fn = moe_sparse if moe_mode == "sparse" else moe_phase

        if "moe0" in phases:
            moe_fn(0, (xT if moe_src == 'xT' else x1T), x2T)

        if "odd" in phases:
            with ExitStack() as es:
                win = c.sb(es, "o_win_sb", [128, 8, 1536], F32R)
                for kc in range(8):
                    c.dma(win[:, kc, :], o_win[kc * 128:(kc + 1) * 128, :], q="gpsimd")
                wo = c.sb(es, "o_wo", [128, 8, 1024], F32R)
                c.dma(wo[:], o_wout[:, :].re("(kc p) f -> p kc f", p=128), q="gpsimd")
                pw = c.sb(es, "o_pw", [128, 4, 128], F32R)
                c.dma(pw[:], poolw_d[:], q="gpsimd")
                psc = c.sb(es, "o_psc", [128, 4])
                c.dma(psc[:], pools_d[:])
                sgg = c.sb(es, "o_sgg", [128, 512])
                c.dma(sgg[:], sgug_d[:])
                sw = c.sb(es, "o_sw", [128, 4, 128], F32R)
                with ExitStack() as es2:
                    swf = c.sb(es2, "o_swf", [128, 4, 128])
                    c.dma(swf[:], sguw_d[:])
                    cm = c.sb(es2, "o_cm", [128, 128])
                    c.dma(cm[:], cmask_d[:, 0, 0:128])
                    for hh in range(4):
                        c.tt(sw[:, hh, :], swf[:, hh, :], cm[:, :], ALU.mult)
                    c.barrier()
                sbb = c.sb(es, "o_sb", [1, 512], F32R)
                c.dma(sbb[:], sgub_d[:], q="gpsimd")
                i16 = c.sb(es, "o_i16", [128, 4, 16])
                c.dma(i16[:], inv16_d[:])
                st = mk_norm(es, "on")
                xt_ = c.sb(es, "o_x", [128, 8, 512])
                h = c.sb(es, "o_h", [128, 8, 512], F32R)
                ub = c.sb(es, "o_ub", [128, 4, 528])
                s2 = c.sb(es, "o_s2", [128, 528])
                s3 = c.sb(es, "o_s3", [128, 528])
                mix = c.sb(es, "o_mix", [128, 8, 512], F32R)
                ug = c.sb(es, "o_ug", [128, 4, 512])
                zt4 = c.sb(es, "o_zt4", [128, 4, 512])
                g14 = c.sb(es, "o_g14", [128, 4, 512])
                vss4 = c.sb(es, "o_vss4", [128, 4])
                vt4 = c.sb(es, "o_vt4", [128, 4, 512], F32R)
                pl_ = c.sb(es, "o_pl", [128, 512], F32R)
                c.memset(ub[:], 0.0)
                wins = (2, 4, 8, 16)
                for i in range(NT):
                    c.dma(xt_[:], fm((xT if odd_src == 'xT' else x2T), i, 8))
                    norm_tile(st, xt_, h, gg[:, 2, :], mod[:, 48:56], P[7])
                    for gi in range(4):
                        pb = P[gi % 2]
                        for kc in range(8):
                            c.mm(pb[:, :], win[:, kc, gi * 128:(gi + 1) * 128], h[:, kc, :], start=(kc == 0), stop=(kc == 7))
                        c.copy(ub[:, gi, 16:528], pb[:, :], eng="scalar")
                        w = wins[gi]
                        cur = ub[:, gi, :]
                        k = 1
                        dst = s2
                        while k < w:
                            nxt = dst[:, :]
                            c.tt(nxt[:, 2 * k - 1:528], cur[:, 2 * k - 1:528], cur[:, k - 1:528 - k], ALU.add)
                            cur = nxt
                            dst = s3 if dst is s2 else s2
                            k *= 2
                        c.stt(pl_[:, :], cur[:, 16:528], 1.0 / w, ub[:, gi, 16:528], ALU.mult, ALU.subtract)
                        if i == 0:
                            c.tt(zt4[:, 0, 0:16], cur[:, 16:32], i16[:, gi, :], ALU.mult)
                            c.tt(pl_[:, 0:16], zt4[:, 0, 0:16], ub[:, gi, 16:32], ALU.subtract)
                        c.mm(P[2 + gi % 2][:, :], pw[:, gi, :], pl_[:, :])
                        c.ts(mix[:, gi, :], P[2 + gi % 2][:, :], psc[:, gi:gi + 1])
                        c.copy(ub[:, gi, 0:16], ub[:, gi, 512:528], eng="scalar")
                    for m in range(4):
                        for kc in range(8):
                            c.mm(P[m][:, :], win[:, kc, 512 + m * 128:512 + (m + 1) * 128], h[:, kc, :], start=(kc == 0), stop=(kc == 7))
                        c.copy(zt4[:, m, :], P[m][:, :], eng="scalar")
                    gelu_tanh_n(c, [ug[:, m, :] for m in range(4)], [zt4[:, m, :] for m in range(4)],
                                [g14[:, m, :] for m in range(4)], [g14[:, m, :] for m in range(4)])
                    for s_ in range(4):
                        ts_ = slice(s_ * 128, (s_ + 1) * 128)
                        for kc in range(8):
                            c.mm(P[4 + s_][:, :], h[:, kc, ts_], win[:, kc, 1024:1536], start=(kc == 0), stop=(kc == 7))
                        c.copy(zt4[:, s_, :], P[4 + s_][:, :], eng="scalar")
                    gelu_tanh_n(c, [g14[:, q, :] for q in range(4)], [zt4[:, q, :] for q in range(4)],
                                [g14[:, q, :] for q in range(4)], [g14[:, q, :] for q in range(4)])
                    for q in range(4):
                        c.tt(zt4[:, q, :], g14[:, q, :], g14[:, q, :], ALU.mult)
                    for q in range(4):
                        c.rsum(vss4[:, q:q + 1], zt4[:, q, :])
                    c.ts(vss4[:, :], vss4[:, :], 1.0 / 512, EPS, ALU.mult, ALU.add)
                    c.act(vss4[:, :], vss4[:, :], AF.Sqrt)
                    c.recip(vss4[:, :], vss4[:, :])
                    for q in range(4):
                        c.stt(vt4[:, q, :], g14[:, q, :], vss4[:, q:q + 1], sgg[:, :], ALU.mult, ALU.mult)
                    for q in range(4):
                        for hh in range(4):
                            c.mm(P[4 + q][:, hh * 128:(hh + 1) * 128], vt4[:, q, hh * 128:(hh + 1) * 128], sw[:, hh, :], start=True, stop=False)
                            c.mm(P[4 + q][:, hh * 128:(hh + 1) * 128], ones_r[0:1, :], sbb[0:1, hh * 128:(hh + 1) * 128], start=False, stop=True)
                    for q in range(4):
                        for hh in range(4):
                            c.tt(mix[:, 4 + hh, q * 128:(q + 1) * 128], ug[:, hh, q * 128:(q + 1) * 128], P[4 + q][:, hh * 128:(hh + 1) * 128], ALU.mult)
                    for dc in range(8):
                        pb = P[dc % 4]
                        for kc in range(8):
                            c.mm(pb[:, :], wo[:, kc, dc * 128:(dc + 1) * 128], mix[:, kc, :], start=(kc == 0), stop=(kc == 7))
                        c.stt(xt_[:, dc, :], pb[:, :], mod[:, 48 + 16 + dc:48 + 17 + dc], xt_[:, dc, :], ALU.mult, ALU.add)
                    c.dma(fm(x3T, i, 8), xt_[:])
                c.barrier()

        if "moe1" in phases:
            moe_fn(1, x3T, outT)
        c.barrier()
    return nc


def host_inputs(inp, b, S):
    f = np.float32
    g = lambda k: np.asarray(inp[k])
    chunkT = lambda v, n: np.ascontiguousarray(v.reshape(n, 128).T)
    m = {}
    m["xT"] = np.ascontiguousarray(g("x")[b, :S].T)
    m["cT"] = chunkT(g("c")[b], 8)
    m["pos"] = np.ascontiguousarray(g("positions")[b:b + 1, :S]).astype(np.int32)
    m["ada_w"] = g("ada_w")
    m["ada_bT"] = chunkT(g("ada_b").reshape(-1), 96)
    m["nmg"] = chunkT(g("norm_mix_g").reshape(-1), 16)
    m["nfg"] = chunkT(g("norm_ffn_g").reshape(-1), 16)
    m["router_w"] = g("router_w")
    m["router_b"] = g("router_b")
    wg = g("moe_w_gu")
    wg = wg.reshape(2, NE, 8, 128, 2, 8, 128)
    wg = wg.reshape(2, NE, 8, 128, 2, 4, 2, 128)
    m["wgu"] = np.ascontiguousarray(wg.transpose(0, 1, 5, 3, 6, 2, 4, 7)).reshape(2 * NE * 4 * 128, 4096)
    m["bgu"] = np.ascontiguousarray(g("moe_b_gu").reshape(2, NE, 16, 128).transpose(0, 3, 1, 2))
    m["bgu2"] = np.ascontiguousarray(g("moe_b_gu").reshape(2, NE, 16, 128).transpose(0, 1, 3, 2)).reshape(2, NE * 128, 16)
    m["bdnr"] = g("moe_b_dn").reshape(2 * NE, D)
    m["bdn2"] = np.ascontiguousarray(g("moe_b_dn").reshape(2, NE, 8, 128).transpose(0, 1, 3, 2)).reshape(2, NE * 128, 8)
    NSUB = S // 128
    NBLK = (4 * S + NE * 511 + 511) // 512
    pp = np.arange(128)
    m["ustrict"] = (pp[None, :] > pp[:, None]).astype(f)
    m["jvec"] = np.broadcast_to(np.arange(NBLK, dtype=f)[None, :], (128, NBLK))
    m["eiota"] = np.broadcast_to(np.arange(NE, dtype=f)[None, :], (128, NE))
    m["tokid"] = (np.arange(NSUB)[None, :] * 128 + pp[:, None]).astype(f)
    wi = np.zeros((128, 9), f)
    wi[:, 0:8] = np.arange(8)[None, :] * 128 + pp[:, None]
    wi[:, 8] = pp
    m["widx"] = wi
    rinit = np.zeros((128, NBLK * 4, 2), f)
    rinit[:, :, 0] = S + (np.arange(NBLK * 4)[None, :] % 4) * 128 + pp[:, None]
    m["rowinit"] = rinit
    wd_ = g("moe_w_dn").reshape(2, NE, 2, 4, 128, D)
    m["wdn"] = np.ascontiguousarray(wd_.transpose(0, 1, 2, 4, 3, 5)).reshape(2 * NE * 2 * 128, 4096)
    m["bdn"] = g("moe_b_dn")
    m["e_win"] = g("even_w_in")[0]
    m["qng"] = chunkT(g("mla_q_norm_g")[0], 2)
    m["kvng"] = chunkT(g("mla_kv_norm_g")[0], 1)
    wuq = g("mla_w_uq")[0]
    perm = np.concatenate([np.arange(64), np.arange(80, 96), np.arange(64, 80)])
    lay = lambda w: np.ascontiguousarray(w.reshape(2, 128, 8, 96).transpose(2, 1, 0, 3))
    m["wuq"] = lay(wuq)
    m["wuqs"] = lay(wuq[:, :, perm])
    wukv = g("mla_w_ukv")[0]
    m["wk"] = np.ascontiguousarray(wukv[:, :, 0:64].transpose(1, 0, 2))
    m["wv"] = np.ascontiguousarray(wukv[:, :, 64:128].transpose(1, 0, 2))
    qh = g("mla_q_head_g")[0]
    kh = g("mla_k_head_g")[0]
    m["qhg"] = np.ascontiguousarray(np.stack([qh, qh[perm]], 1))
    m["khg"] = np.ascontiguousarray(np.stack([kh, kh[perm]], 1))
    m["ident"] = np.eye(128, dtype=f)
    ipe = np.zeros((32, 2, 96), f)
    for j in range(32):
        ipe[j, 0, 64 + j] = 1.0
        ipe[(j + 16) % 32, 1, 64 + j] = 1.0
    m["ipe"] = ipe
    rc = np.zeros((96, 2), f)
    half = 16
    invf = (1.0 / (10000.0 ** (np.arange(half, dtype=np.float32) / half))).astype(f)
    rc[64:80, 0] = invf
    rc[80:96, 0] = invf
    rc[64:80, 1] = -1.0
    rc[80:96, 1] = 1.0
    m["ropec"] = rc
    sel = np.zeros((NE, NE, 128), f)
    for e in range(NE):
        sel[e, e, :] = 1.0
    m["sel"] = sel
    s65 = np.zeros((65, 64), f)
    s65[64, :] = 1.0
    m["sel65"] = s65
    cm = np.zeros((128, 4, 512), f)
    kk = np.arange(128)[:, None]
    qq = np.arange(512)[None, :]
    for j in range(4):
        cm[:, j, :] = (qq >= kk + 128 * j)
    m["cmask"] = cm
    i16 = np.zeros((128, 4, 16), f)
    for gi, w in enumerate((2, 4, 8, 16)):
        i16[:, gi, :] = 1.0 / np.minimum(np.arange(16) + 1, w)
    m["inv16"] = i16
    sm = lambda a: np.ascontiguousarray(a.reshape(16, 128, -1).transpose(1, 0, 2))
    a_re, a_im = g("s5_a_re")[0].reshape(-1), g("s5_a_im")[0].reshape(-1)
    ldt = np.repeat(g("s5_log_dt")[0], 64)
    m["s5p"] = np.ascontiguousarray(np.stack([chunkT(a_re, 16), chunkT(a_im, 16), chunkT(ldt, 16)], 1))
    br, bi = g("s5_b_re")[0].reshape(2048, 16), g("s5_b_im")[0].reshape(2048, 16)
    m["s5b"] = np.ascontiguousarray(np.stack([sm(br), sm(bi)], 1))
    cr = g("s5_c_re")[0].transpose(0, 2, 1).reshape(2048, 16)
    ci = g("s5_c_im")[0].transpose(0, 2, 1).reshape(2048, 16)
    m["s5c"] = np.ascontiguousarray(np.stack([sm(cr), sm(ci)], 1))
    m["s5d"] = chunkT(g("s5_d")[0], 4)
    m["gluw"] = g("s5_glu_w")[0]
    m["glub"] = chunkT(g("s5_glu_b")[0], 4)
    m["e_wout"] = g("even_w_out")[0]
    m["o_win"] = g("odd_w_in")[0]
    m["poolw"] = np.ascontiguousarray(g("pool_w")[0].transpose(1, 0, 2))
    m["pools"] = chunkT(g("pool_scale")[0], 4)
    m["sgug"] = np.ascontiguousarray(np.broadcast_to(g("sgu_norm_g")[0][None, :], (128, 512)))
    m["sguw"] = np.ascontiguousarray(g("sgu_w")[0].transpose(2, 0, 1))
    m["sgub"] = np.ascontiguousarray(g("sgu_b")[0].reshape(1, 512))
    m["o_wout"] = g("odd_w_out")[0]
    return {k: np.ascontiguousarray(v, dtype=(np.int32 if k == "pos" else f)) for k, v in m.items()}


def kernel(**inputs):
    B, S, _ = inputs["x"].shape
    nc = build(S)
    m0 = host_inputs(inputs, 0, S)
    maps = [m0]
    f = np.float32
    for b in range(1, B):
        mb = dict(m0)
        mb["xT"] = np.ascontiguousarray(np.asarray(inputs["x"])[b, :S].T, dtype=f)
        mb["cT"] = np.ascontiguousarray(np.asarray(inputs["c"])[b].reshape(8, 128).T, dtype=f)
        mb["pos"] = np.ascontiguousarray(np.asarray(inputs["positions"])[b:b + 1, :S]).astype(np.int32)
        maps.append(mb)
    res = run_bass_kernel_spmd(nc, maps, core_ids=list(range(B)))
    out = np.stack([np.ascontiguousarray(res.results[b]["outT"].T) for b in range(B)], 0)
    return out.astype(np.float32)
```
